# Optimizing a Trainium2 kernel written in Bass

```python
import jax, jax.numpy as jnp
from jax import lax
import numpy as np

D_MODEL = 1024
BATCH = 16
SEQ = 4096
DEPTH = 4

GRID_W = 64
CTX_LEN = 256
N_MIXERS = 2
N_MLSTM_LAYERS = (DEPTH + 1) // 2
N_ATT_LAYERS = DEPTH // 2
NORM_EPS = 1e-6

MLSTM_HEADS = 4
MLSTM_QK_DIM = D_MODEL // 2
MLSTM_V_DIM = D_MODEL
MLSTM_DK = MLSTM_QK_DIM // MLSTM_HEADS
MLSTM_DV = MLSTM_V_DIM // MLSTM_HEADS
MLSTM_CHUNK = 64
MLSTM_GATE_CAP = 15.0
MLSTM_N_GATES = 4
MLSTM_IN_DIM = 2 * MLSTM_QK_DIM + 2 * MLSTM_V_DIM + MLSTM_N_GATES * MLSTM_HEADS

ATT_HEADS = 16
ATT_KV_HEADS = 4
ATT_GROUP = ATT_HEADS // ATT_KV_HEADS
ATT_HEAD_DIM = D_MODEL // ATT_HEADS
ATT_Q_BLOCK = 128
ATT_IN_DIM = (ATT_HEADS + 2 * ATT_KV_HEADS) * ATT_HEAD_DIM
ROPE_THETA = 10000.0
ROPE_PAIRS_PER_AXIS = ATT_HEAD_DIM // 4

N_EXPERTS = 16
EC_CAPACITY = 2
D_EXPERT = D_MODEL

kernel_name = 'hybrid_mlstm_gqa_ec_moe_dit'


def _rmsnorm(x, g):
    xf = x.astype(jnp.float32)
    y = xf * lax.rsqrt(jnp.mean(xf * xf, axis=-1, keepdims=True) + NORM_EPS)
    return (y * g.astype(jnp.float32)).astype(x.dtype)


def _modulate(h, shift, scale):
    return h * (1 + scale) + shift


def _axial_rope(n_tok):
    rows = n_tok // GRID_W
    row = jnp.repeat(jnp.arange(rows, dtype=jnp.float32), GRID_W)
    col = jnp.tile(jnp.arange(GRID_W, dtype=jnp.float32), rows)
    inv = ROPE_THETA ** (-jnp.arange(ROPE_PAIRS_PER_AXIS, dtype=jnp.float32) / ROPE_PAIRS_PER_AXIS)
    ang = jnp.concatenate([row[:, None] * inv, col[:, None] * inv], axis=-1)
    return jnp.cos(ang), jnp.sin(ang)


def _apply_rope(x, cos, sin):
    shape = (x.shape[1],) + (1,) * (x.ndim - 3) + (cos.shape[-1],)
    cos = cos.reshape(shape).astype(x.dtype)
    sin = sin.reshape(shape).astype(x.dtype)
    x1, x2 = jnp.split(x, 2, axis=-1)
    return jnp.concatenate([x1 * cos - x2 * sin, x1 * sin + x2 * cos], axis=-1)


def _mlstm_zero_state(bsz):
    return (jnp.zeros((bsz, MLSTM_HEADS, MLSTM_DK, MLSTM_DV), jnp.float32),
            jnp.zeros((bsz, MLSTM_HEADS, MLSTM_DK), jnp.float32),
            jnp.zeros((bsz, MLSTM_HEADS), jnp.float32))


def _mlstm_scan(q, k, v, ig, lf, state):
    bsz, nh, n_tok, _ = q.shape
    n_chunks = n_tok // MLSTM_CHUNK
    causal = jnp.tril(jnp.ones((MLSTM_CHUNK, MLSTM_CHUNK), dtype=bool))

    def to_chunks(a):
        a = a.reshape(a.shape[:2] + (n_chunks, MLSTM_CHUNK) + a.shape[3:])
        return jnp.moveaxis(a, 2, 0)

    def step(carry, inp):
        c_mat, n_vec, m_st = carry
        qc, kc, vc, igc, lfc = inp
        b = jnp.cumsum(lfc, axis=-1)
        log_d = jnp.where(causal, b[..., :, None] - b[..., None, :] + igc[..., None, :], -jnp.inf)
        m_inter = b + m_st[..., None]
        m_q = jnp.maximum(m_inter, jnp.max(log_d, axis=-1))
        w_intra = jnp.exp(log_d - m_q[..., None])
        w_inter = jnp.exp(m_inter - m_q)
        s = jnp.einsum('bhjd,bhsd->bhjs', qc, kc) * w_intra
        num = jnp.einsum('bhjs,bhsv->bhjv', s, vc) + w_inter[..., None] * jnp.einsum('bhjd,bhdv->bhjv', qc, c_mat)
        den = jnp.sum(s, axis=-1) + w_inter * jnp.einsum('bhjd,bhd->bhj', qc, n_vec)
        h = num / jnp.maximum(jnp.abs(den), jnp.exp(-m_q))[..., None]
        b_last = b[..., -1]
        log_w = b_last[..., None] - b + igc
        m_new = jnp.maximum(b_last + m_st, jnp.max(log_w, axis=-1))
        w_key = jnp.exp(log_w - m_new[..., None])
        decay = jnp.exp(b_last + m_st - m_new)
        c_new = decay[..., None, None] * c_mat + jnp.einsum('bhsd,bhsv->bhdv', kc * w_key[..., None], vc)
        n_new = decay[..., None] * n_vec + jnp.einsum('bhs,bhsd->bhd', w_key, kc)
        return (c_new, n_new, m_new), h

    state, h = lax.scan(step, state, tuple(to_chunks(a) for a in (q, k, v, ig, lf)))
    h = jnp.moveaxis(h, 0, 2).reshape(bsz, nh, n_tok, MLSTM_DV)
    return h, state


def _bidir_mlstm(qkv, fwd, bwd, state_f, state_b):
    flip = lambda a: jnp.flip(a, axis=2)
    h_f, state_f = _mlstm_scan(*qkv, *fwd, state_f)
    h_b, state_b = _mlstm_scan(*(flip(a) for a in qkv), *(flip(a) for a in bwd), state_b)
    return h_f + flip(h_b), state_f, state_b


def _mlstm_mixer(h_ctx, h_lat, w_in, b_gate, w_norm, w_out, ctx_out):
    splits = [MLSTM_QK_DIM, 2 * MLSTM_QK_DIM, 2 * MLSTM_QK_DIM + MLSTM_V_DIM, 2 * MLSTM_QK_DIM + 2 * MLSTM_V_DIM]

    def project(h):
        bsz, n_tok, _ = h.shape
        q, k, v, o, g = jnp.split(h @ w_in, splits, axis=-1)

        def heads(a, d):
            return a.reshape(bsz, n_tok, MLSTM_HEADS, d).transpose(0, 2, 1, 3).astype(jnp.float32)

        q = heads(q, MLSTM_DK) * (MLSTM_DK ** -0.5)
        k = heads(k, MLSTM_DK)
        v = heads(v, MLSTM_DV)
        g = (g + b_gate).astype(jnp.float32)
        g = MLSTM_GATE_CAP * jnp.tanh(g / MLSTM_GATE_CAP)
        g = g.reshape(bsz, n_tok, MLSTM_N_GATES, MLSTM_HEADS).transpose(2, 0, 3, 1)
        fwd = (g[0], jax.nn.log_sigmoid(g[1]))
        bwd = (g[2], jax.nn.log_sigmoid(g[3]))
        return (q, k, v), o, fwd, bwd

    def readout(hh, o):
        bsz, _, n_tok, _ = hh.shape
        hh = _rmsnorm(hh, w_norm.reshape(MLSTM_HEADS, 1, MLSTM_DV))
        hh = hh.transpose(0, 2, 1, 3).reshape(bsz, n_tok, MLSTM_V_DIM).astype(o.dtype)
        return (hh * jax.nn.sigmoid(o)) @ w_out

    qkv_c, o_c, fwd_c, bwd_c = project(h_ctx)
    qkv_l, o_l, fwd_l, bwd_l = project(h_lat)
    zero = _mlstm_zero_state(h_ctx.shape[0])
    hh_c, st_f, st_b = _bidir_mlstm(qkv_c, fwd_c, bwd_c, zero, zero)
    hh_l, _, _ = _bidir_mlstm(qkv_l, fwd_l, bwd_l, st_f, st_b)
    y_lat = readout(hh_l, o_l)
    y_ctx = readout(hh_c, o_c) if ctx_out else None
    return y_ctx, y_lat


def _gqa_mixer(h_ctx, h_lat, w_in, q_norm, k_norm, w_out, cos, sin, ctx_out):
    scale = ATT_HEAD_DIM ** -0.5

    def project(h):
        bsz, n_tok, _ = h.shape
        q, k, v = jnp.split(h @ w_in, [ATT_HEADS * ATT_HEAD_DIM, (ATT_HEADS + ATT_KV_HEADS) * ATT_HEAD_DIM], axis=-1)
        q = _rmsnorm(q.reshape(bsz, n_tok, ATT_KV_HEADS, ATT_GROUP, ATT_HEAD_DIM), q_norm)
        k = _rmsnorm(k.reshape(bsz, n_tok, ATT_KV_HEADS, ATT_HEAD_DIM), k_norm)
        v = v.reshape(bsz, n_tok, ATT_KV_HEADS, ATT_HEAD_DIM)
        return q, k, v

    def attend(q, k, v):
        s = jnp.einsum('bqgrd,bkgd->bgrqk', q, k).astype(jnp.float32) * scale
        p = jax.nn.softmax(s, axis=-1).astype(v.dtype)
        return jnp.einsum('bgrqk,bkgd->bqgrd', p, v)

    bsz, n_lat, _ = h_lat.shape
    q_c, k_c, v_c = project(h_ctx)
    q_l, k_l, v_l = project(h_lat)
    q_l = _apply_rope(q_l, cos, sin)
    k_l = _apply_rope(k_l, cos, sin)
    k_all = jnp.concatenate([k_c, k_l], axis=1)
    v_all = jnp.concatenate([v_c, v_l], axis=1)
    n_blocks = n_lat // ATT_Q_BLOCK
    q_blocks = jnp.moveaxis(q_l.reshape(bsz, n_blocks, ATT_Q_BLOCK, ATT_KV_HEADS, ATT_GROUP, ATT_HEAD_DIM), 1, 0)
    o_l = lax.map(lambda qb: attend(qb, k_all, v_all), q_blocks)
    o_l = jnp.moveaxis(o_l, 0, 1).reshape(bsz, n_lat, ATT_HEADS * ATT_HEAD_DIM)
    y_lat = o_l @ w_out
    y_ctx = attend(q_c, k_c, v_c).reshape(bsz, h_ctx.shape[1], ATT_HEADS * ATT_HEAD_DIM) @ w_out if ctx_out else None
    return y_ctx, y_lat


def _ec_moe(h, w_router, w_gate, w_up, w_down):
    bsz, n_tok, d = h.shape
    cap = max(1, EC_CAPACITY * n_tok // N_EXPERTS)
    aff = jax.nn.softmax((h @ w_router).astype(jnp.float32), axis=-1)
    g, idx = lax.top_k(jnp.swapaxes(aff, 1, 2), cap)
    xg = jax.vmap(lambda hb, ib: hb[ib])(h, idx)
    a = jnp.einsum('becd,edf->becf', xg, w_gate)
    u = jnp.einsum('becd,edf->becf', xg, w_up)
    y = jnp.einsum('becf,efd->becd', jax.nn.silu(a) * u, w_down) * g[..., None].astype(h.dtype)
    return jax.vmap(lambda ib, yb: jnp.zeros((n_tok, d), yb.dtype).at[ib.reshape(-1)].add(yb.reshape(-1, d)))(idx, y)


def setup_inputs(seed: int = 0) -> dict:
    key = jax.random.key(seed)
    ks = iter(jax.random.split(key, 24))

    def nrm(shape, scale):
        return scale * jax.random.normal(next(ks), shape, jnp.float32)

    def gain(shape):
        return 1.0 + nrm(shape, 0.05)

    inv_d = D_MODEL ** -0.5
    att_w = ATT_HEADS * ATT_HEAD_DIM
    b_gate_base = jnp.tile(jnp.array([0.0, 3.0, 0.0, 3.0], jnp.float32)[:, None], (1, MLSTM_HEADS)).reshape(-1)
    return {
        'x': nrm((BATCH, SEQ, D_MODEL), 1.0),
        'c': nrm((BATCH, D_MODEL), 1.0),
        'ctx': nrm((BATCH, CTX_LEN, D_MODEL), 1.0),
        'c_ctx': nrm((D_MODEL,), 1.0),
        'w_mod': nrm((DEPTH, D_MODEL, 6 * D_MODEL), 0.5 * inv_d),
        'b_mod': nrm((DEPTH, 6 * D_MODEL), 0.02),
        'norm_mix': gain((DEPTH, D_MODEL)),
        'norm_ffn': gain((DEPTH, D_MODEL)),
        'mlstm_w_in': nrm((N_MLSTM_LAYERS, D_MODEL, MLSTM_IN_DIM), inv_d),
        'mlstm_b_gate': b_gate_base + nrm((N_MLSTM_LAYERS, MLSTM_N_GATES * MLSTM_HEADS), 0.1),
        'mlstm_norm': gain((N_MLSTM_LAYERS, MLSTM_V_DIM)),
        'mlstm_w_out': nrm((N_MLSTM_LAYERS, MLSTM_V_DIM, D_MODEL), MLSTM_V_DIM ** -0.5),
        'attn_w_in': nrm((N_ATT_LAYERS, D_MODEL, ATT_IN_DIM), inv_d),
        'attn_q_norm': gain((N_ATT_LAYERS, ATT_HEAD_DIM)),
        'attn_k_norm': gain((N_ATT_LAYERS, ATT_HEAD_DIM)),
        'attn_w_out': nrm((N_ATT_LAYERS, att_w, D_MODEL), att_w ** -0.5),
        'moe_router': nrm((DEPTH, D_MODEL, N_EXPERTS), inv_d),
        'moe_w_gate': nrm((DEPTH, N_EXPERTS, D_MODEL, D_EXPERT), inv_d),
        'moe_w_up': nrm((DEPTH, N_EXPERTS, D_MODEL, D_EXPERT), inv_d),
        'moe_w_down': nrm((DEPTH, N_EXPERTS, D_EXPERT, D_MODEL), D_EXPERT ** -0.5),
        'norm_final': gain((D_MODEL,)),
    }


def reference(x, c, ctx, c_ctx, w_mod, b_mod, norm_mix, norm_ffn, mlstm_w_in, mlstm_b_gate, mlstm_norm, mlstm_w_out,
              attn_w_in, attn_q_norm, attn_k_norm, attn_w_out, moe_router, moe_w_gate, moe_w_up, moe_w_down, norm_final):
    n_lat = x.shape[1]
    cos, sin = _axial_rope(n_lat)
    silu_c = jax.nn.silu(c)
    silu_cc = jax.nn.silu(c_ctx)
    xc = ctx
    for i in range(DEPTH):
        last = i == DEPTH - 1
        mod_l = (silu_c @ w_mod[i] + b_mod[i])[:, None, :]
        mod_c = silu_cc @ w_mod[i] + b_mod[i]
        sh1_l, sc1_l, g1_l, sh2_l, sc2_l, g2_l = jnp.split(mod_l, 6, axis=-1)
        sh1_c, sc1_c, g1_c, sh2_c, sc2_c, g2_c = jnp.split(mod_c, 6, axis=-1)
        h_l = _modulate(_rmsnorm(x, norm_mix[i]), sh1_l, sc1_l)
        h_c = _modulate(_rmsnorm(xc, norm_mix[i]), sh1_c, sc1_c)
        j = i // N_MIXERS
        if i % N_MIXERS == 0:
            y_c, y_l = _mlstm_mixer(h_c, h_l, mlstm_w_in[j], mlstm_b_gate[j], mlstm_norm[j], mlstm_w_out[j], not last)
        else:
            y_c, y_l = _gqa_mixer(h_c, h_l, attn_w_in[j], attn_q_norm[j], attn_k_norm[j], attn_w_out[j], cos, sin, not last)
        x = x + g1_l * y_l
        h_l = _modulate(_rmsnorm(x, norm_ffn[i]), sh2_l, sc2_l)
        x = x + g2_l * _ec_moe(h_l, moe_router[i], moe_w_gate[i], moe_w_up[i], moe_w_down[i])
        if not last:
            xc = xc + g1_c * y_c
            h_c = _modulate(_rmsnorm(xc, norm_ffn[i]), sh2_c, sc2_c)
            xc = xc + g2_c * _ec_moe(h_c, moe_router[i], moe_w_gate[i], moe_w_up[i], moe_w_down[i])
    return _rmsnorm(x, norm_final)
```

```python
import contextlib
import numpy as np
import concourse.bass as bass
import concourse.mybir as mybir
from concourse.bass_utils import run_bass_kernel_spmd

F32 = mybir.dt.float32
BF16 = mybir.dt.bfloat16
U32 = mybir.dt.uint32
I32 = mybir.dt.int32
AF = mybir.ActivationFunctionType
ALU = mybir.AluOpType
AX = mybir.AxisListType

D = 1024
NL = 4096
NCX = 256
NT = NL + NCX
DEPTH = 4
EPS = 1e-6
NE = 16
ND = 6


class Buf:
    __slots__ = ("w", "r", "excl")

    def __init__(self, excl=False):
        self.w = {}
        self.r = {}
        self.excl = excl


class Scope:
    def __init__(self, fw):
        self.fw = fw
        self.es = contextlib.ExitStack()

    def __enter__(self):
        self.es.__enter__()
        return self

    def __exit__(self, *a):
        self.fw.barrier()
        return self.es.__exit__(*a)

    def sb(self, name, shape, dt):
        self.fw.uid += 1
        t = self.es.enter_context(self.fw.nc.sbuf_tensor(f"{name}_{self.fw.uid}", list(shape), dt))
        return t, Buf()

    def ps(self, name, shape, dt=F32):
        self.fw.uid += 1
        esz = 4 if dt == F32 else 2
        bank = 2048 // esz
        n = 1
        for d_ in shape[1:]:
            n *= d_
        nalloc = ((n + bank - 1) // bank) * bank
        t = self.es.enter_context(self.fw.nc.psum_tensor(f"{name}_{self.fw.uid}", [128, nalloc], dt))
        v = t[0:shape[0], 0:n]
        if len(shape) == 3:
            v = v.rearrange("p (a b) -> p a b", a=shape[1], b=shape[2])
        elif len(shape) != 2:
            raise ValueError(shape)
        return v, Buf(excl=True)


class FW:
    def __init__(self, nc, es):
        self.nc = nc
        self.es = es
        self.uid = 0
        self.e = dict(pe=nc.tensor, act=nc.scalar, pool=nc.gpsimd, dve=nc.vector, sp=nc.sync)
        self.semh = {}
        self.cnt = {}
        self.seen = {k: {} for k in self.e}
        for k in self.e:
            key = "c_" + k
            self.semh[key] = es.enter_context(nc.semaphore(key))
            self.cnt[key] = 0
        self.drr = {}
        for q in ("sp", "pool", "act", "poolw"):
            self.drr[q] = 0
            for i in range(ND):
                key = f"d_{q}{i}"
                self.semh[key] = es.enter_context(nc.semaphore(key))
                self.cnt[key] = 0
        self.n_ins = 0
        self.n_wait = 0

    def scope(self):
        return Scope(self)

    def _wait(self, eng, deps):
        seen = self.seen[eng]
        for k, v in deps.items():
            if seen.get(k, 0) >= v:
                continue
            self.e[eng].wait_ge(self.semh[k], v)
            seen[k] = v
            self.n_wait += 1

    def op(self, eng, fn, reads=(), writes=()):
        deps = {}
        own = "c_" + eng
        for b in reads:
            for k, v in b.w.items():
                if deps.get(k, 0) < v:
                    deps[k] = v
            if b.excl:
                for k, v in b.r.items():
                    if k != own and deps.get(k, 0) < v:
                        deps[k] = v
        for b in writes:
            for src in (b.w, b.r):
                for k, v in src.items():
                    if k == own:
                        continue
                    if deps.get(k, 0) < v:
                        deps[k] = v
        self._wait(eng, deps)
        ins = fn(self.e[eng])
        self.cnt[own] += 1
        ins.then_inc(self.semh[own], 1)
        v = self.cnt[own]
        for b in reads:
            b.r[own] = v
        for b in writes:
            b.w[own] = v
        self.n_ins += 1
        return ins

    def dma(self, q, fn, reads=(), writes=(), waw=True):
        deps = {}
        for b in reads:
            for k, v in b.w.items():
                if deps.get(k, 0) < v:
                    deps[k] = v
        for b in writes:
            for src in ((b.w, b.r) if waw else (b.r,)):
                for k, v in src.items():
                    if deps.get(k, 0) < v:
                        deps[k] = v
        i = self.drr[q]
        self.drr[q] = (i + 1) % ND
        key = f"d_{q}{i}"
        if self.cnt[key] > 0 and deps.get(key, 0) < self.cnt[key]:
            deps[key] = self.cnt[key]
        q = "pool" if q == "poolw" else q
        self._wait(q, deps)
        ins = fn(self.e[q])
        self.cnt[key] += 16
        ins.then_inc(self.semh[key], 16)
        v = self.cnt[key]
        for b in reads:
            b.r[key] = v
        for b in writes:
            b.w[key] = v
        self.n_ins += 1
        return ins

    def barrier(self):
        allv = {k: v for k, v in self.cnt.items() if v > 0}
        for eng in self.e:
            self._wait(eng, allv)

    def finish(self):
        self._wait("sp", {k: v for k, v in self.cnt.items() if v > 0})


MOD_OFF = dict(sh1=0, sc1=1, g1=2, sh2=3, sc2=4, g2=5)


class Prog:
    def __init__(self, nb, plan, dbg=False):
        self.nb = nb
        self.plan = plan
        nc = self.nc = bass.Bass("TRN2", target_bir_lowering=False)
        dt = nc.dram_tensor

        def ein(name, shape, d=F32):
            return dt(name, list(shape), d, kind="ExternalInput").ap()

        self.x = ein("x", [nb, NL, D])
        self.c = ein("c", [nb, D])
        self.ctx = ein("ctx", [nb, NCX, D])
        self.c_ctx = ein("c_ctx", [1, D])
        self.w_mod = ein("w_mod", [DEPTH, D, 6 * D])
        self.b_mod = ein("b_mod", [DEPTH, 6 * D])
        self.norm_mix = ein("norm_mix", [DEPTH, D])
        self.norm_ffn = ein("norm_ffn", [DEPTH, D])
        self.mlstm_w_in = ein("mlstm_w_in", [2, D, 3088])
        self.mlstm_b_gate = ein("mlstm_b_gate", [2, 16])
        self.mlstm_norm = ein("mlstm_norm", [2, D])
        self.mlstm_w_out = ein("mlstm_w_out", [2, D, D])
        self.attn_w_in = ein("attn_w_in", [2, D, 1536])
        self.attn_q_norm = ein("attn_q_norm", [2, 64])
        self.attn_k_norm = ein("attn_k_norm", [2, 64])
        self.attn_w_out = ein("attn_w_out", [2, D, D])
        self.moe_router = ein("moe_router", [DEPTH, D, NE])
        self.moe_w_gate = ein("moe_w_gate", [DEPTH, NE, D, D])
        self.moe_w_up = ein("moe_w_up", [DEPTH, NE, D, D])
        self.moe_w_down = ein("moe_w_down", [DEPTH, NE, D, D])
        self.norm_final = ein("norm_final", [1, D])
        self.rope_cos = ein("rope_cos", [128, NL])
        self.rope_sin = ein("rope_sin", [128, NL])
        self.outs = [dt(f"out{b}", [NL, D], F32, kind="ExternalOutput").ap() for b in range(nb)]
        self.b_outs = [Buf() for _ in range(nb)]
        self.xcs = [dt(f"xc_res{b}", [NCX, D], F32).ap() for b in range(nb)]
        self.b_xcs = [Buf() for _ in range(nb)]
        self.mod_d = dt("mod_d", [DEPTH, 3, 6 * D], F32).ap()
        self.b_mod_d = Buf()
        self.h2l = [dt(f"h2l{b}", [NL, D], BF16).ap() for b in range(nb)]
        self.h2c = [dt(f"h2c{b}", [NCX, D], BF16).ap() for b in range(nb)]
        self.b_h2 = Buf()
        self.tkv = dt("tkv", [2 * nb, NE, 512], F32).ap()
        self.tki = dt("tki", [2 * nb, NE, 512], U32).ap()
        self.b_tk = Buf()
        self.dbg = dbg
        if dbg:
            self.xc_out = dt("xc_out", [nb, NCX, D], F32, kind="ExternalOutput").ap()
            self.mod_out = dt("mod_out", [DEPTH, 3, 6 * D], F32, kind="ExternalOutput").ap()

        with contextlib.ExitStack() as es:
            fw = self.fw = FW(nc, es)
            self.ms = None
            self.bg = None
            self.consts(es)
            self.phase_init()
            for step in plan:
                if step[0] == "mod":
                    self.phase_mod()
                elif step[0] == "moe":
                    self.phase_moe(step[1])
                elif step[0] == "mix":
                    l_ = step[1]
                    overlap = False
                    for bi in range(nb):
                        if bi == 1 and overlap:
                            self.moe_begin(l_)
                            self.moe_route(0)
                            self.bg = self.moe_topk_gen([0])
                        if l_ % 2 == 1:
                            self.phase_attn(l_, bi)
                        else:
                            self.phase_mlstm(l_, bi)
                        self.bg_drain()
                elif step[0] == "final":
                    self.phase_final()
            if dbg:
                for b in range(nb):
                    fw.dma("sp", lambda e: e.dma_start(out=self.xc_out[b, :, :], in_=self.xcs[b][:, :]),
                           reads=[self.b_xcs[b]])
                fw.dma("sp", lambda e: e.dma_start(out=self.mod_out[:, :, :], in_=self.mod_d[:, :, :]),
                       reads=[self.b_mod_d])
            fw.finish()
            self.stats = (fw.n_ins, fw.n_wait)

    def res_tile(self, bi, t):
        if t < 2:
            return self.xcs[bi][t * 128:(t + 1) * 128, :], self.b_xcs[bi]
        return self.outs[bi][(t - 2) * 128:(t - 1) * 128, :], self.b_outs[bi]

    def consts(self, es):
        fw = self.fw
        nc = self.nc
        self.ident_bf = es.enter_context(nc.sbuf_tensor("ident_bf", [128, 128], BF16))
        self.ident_f = es.enter_context(nc.sbuf_tensor("ident_f", [128, 128], F32))
        self.b_ident = Buf()
        for t in (self.ident_bf, self.ident_f):
            fw.op("pool", lambda e: e.memset(t[:], 1.0), writes=[self.b_ident])
            fw.op("pool", lambda e: e.affine_select(out=t[:], in_=t[:], pattern=[[-1, 128]],
                                                     compare_op=ALU.is_equal, fill=0.0, base=0,
                                                     channel_multiplier=1),
                  reads=[self.b_ident], writes=[self.b_ident])

    def phase_init(self):
        fw = self.fw
        for bi in range(self.nb):
            for h in range(4):
                fw.dma("sp", lambda e: e.dma_start(out=self.outs[bi][h * 1024:(h + 1) * 1024, :],
                                                   in_=self.x[bi, h * 1024:(h + 1) * 1024, :]),
                       writes=[self.b_outs[bi]], waw=False)
            fw.dma("sp", lambda e: e.dma_start(out=self.xcs[bi][:, :], in_=self.ctx[bi, :, :]), writes=[self.b_xcs[bi]])

    def phase_mod(self):
        fw = self.fw
        nb = self.nb
        with fw.scope() as sc:
            cT, b_cT = sc.sb("cT", [128, 8, 3], F32)
            sT, b_sT = sc.sb("sT", [128, 8, 3], F32)
            ps, b_ps = sc.ps("modps", [128, 512])
            wts = [sc.sb("wmod", [128, 8, 512], F32) for _ in range(2)]
            bts = [sc.sb("bmod", [3, 512], F32) for _ in range(2)]
            mrs = [sc.sb("mrow", [3, 512], F32) for _ in range(2)]
            rows = [self.c[min(r, nb - 1), :] for r in range(2)] + [self.c_ctx[0, :]]
            for r in range(3):
                fw.dma("sp", lambda e: e.dma_start(out=cT[:, :, r], in_=rows[r].rearrange("(k p) -> p k", p=128),
                                                   allow_slow_non_contiguous=True), writes=[b_cT])
            fw.op("act", lambda e: e.activation(out=sT[:], in_=cT[:], func=AF.Silu), reads=[b_cT], writes=[b_sT])
            it = 0
            for l in range(DEPTH):
                for n in range(12):
                    (wt, b_wt), (bt, b_bt), (mr, b_mr) = wts[it % 2], bts[it % 2], mrs[it % 2]
                    it += 1
                    n0 = n * 512
                    fw.dma("sp", lambda e: e.dma_start(
                        out=wt[:], in_=self.w_mod[l, :, n0:n0 + 512].rearrange("(k p) n -> p k n", p=128)),
                        writes=[b_wt])
                    fw.dma("sp", lambda e: e.dma_start(
                        out=bt[:], in_=self.b_mod[l:l + 1, n0:n0 + 512].partition_broadcast(3)), writes=[b_bt])
                    for k in range(8):
                        fw.op("pe", lambda e: e.matmul(ps[0:3, :], lhsT=sT[:, k, :], rhs=wt[:, k, :],
                                                       start=(k == 0), stop=(k == 7)),
                              reads=[b_sT, b_wt], writes=[b_ps])
                    fw.op("dve", lambda e: e.tensor_tensor(out=mr[:], in0=ps[0:3, :], in1=bt[:], op=ALU.add),
                          reads=[b_ps, b_bt], writes=[b_mr])
                    fw.dma("sp", lambda e: e.dma_start(out=self.mod_d[l, :, n0:n0 + 512], in_=mr[:]),
                           reads=[b_mr], writes=[self.b_mod_d])

    def load_row(self, sc, name, src_row):
        t, b = sc.sb(name, [128, D], F32)
        self.fw.dma("sp", lambda e: e.dma_start(out=t[:], in_=src_row.partition_broadcast(128)),
                    reads=[self.b_mod_d], writes=[b])
        return t, b

    def mod_row(self, l, r, which):
        o = MOD_OFF[which] * D
        return self.mod_d[l, r:r + 1, o:o + D]

    def make_arow(self, sc, l, r, which_sc, norm_w_row):
        fw = self.fw
        a, b_a = self.load_row(sc, "arow", self.mod_row(l, r, which_sc))
        nw, b_nw = self.load_row(sc, "nwrow", norm_w_row)
        fw.op("dve", lambda e: e.scalar_tensor_tensor(out=a[:], in0=a[:], scalar=1.0, in1=nw[:],
                                                      op0=ALU.add, op1=ALU.mult),
              reads=[b_a, b_nw], writes=[b_a])
        return a, b_a

    def norm_load(self, W, src_ap, b_src):
        xt, b_xt = W["xt"][W["i"] % 3]
        W["i"] += 1
        self.fw.dma("sp", lambda e: e.dma_start(out=xt[:], in_=src_ap), reads=[b_src], writes=[b_xt])
        return xt, b_xt

    def norm_tile(self, W, xtb, arow, b_arow, srow, b_srow, h_out, b_h):
        fw = self.fw
        xt, b_xt = xtb
        junk, b_junk = W["junk"]
        st, b_st = W["st"]
        tmp, b_tmp = W["tmp"]
        fw.op("act", lambda e: e.activation(out=junk[:], in_=xt[:], func=AF.Square, accum_out=st[:, 0:1]),
              reads=[b_xt], writes=[b_junk, b_st])
        fw.op("dve", lambda e: e.tensor_scalar(out=st[:, 1:2], in0=st[:, 0:1], scalar1=1.0 / D, scalar2=EPS,
                                               op0=ALU.mult, op1=ALU.add), reads=[b_st], writes=[b_st])
        fw.op("act", lambda e: e.activation(out=st[:, 2:3], in_=st[:, 1:2], func=AF.Sqrt), reads=[b_st], writes=[b_st])
        fw.op("dve", lambda e: e.reciprocal(out=st[:, 3:4], in_=st[:, 2:3]), reads=[b_st], writes=[b_st])
        fw.op("dve", lambda e: e.scalar_tensor_tensor(out=tmp[:], in0=xt[:], scalar=st[:, 3:4], in1=arow[:],
                                                      op0=ALU.mult, op1=ALU.mult),
              reads=[b_xt, b_st, b_arow], writes=[b_tmp])
        fw.op("pool", lambda e: e.tensor_tensor(out=h_out, in0=tmp[:], in1=srow[:], op=ALU.add),
              reads=[b_tmp, b_srow], writes=[b_h])
        return xt, b_xt

    def norm_work(self, sc):
        return dict(i=0, xt=[sc.sb("xt", [128, D], F32) for _ in range(3)], junk=sc.sb("junk", [128, D], BF16),
                    st=sc.sb("st", [128, 4], F32), tmp=sc.sb("tmp", [128, D], F32))

    def moe_begin(self, l):
        nb = self.nb
        last = l == DEPTH - 1
        sets = [(bi, 0) for bi in range(nb)] + ([] if last else [(bi, 1) for bi in range(nb)])
        p2 = [si for si, (bi, c_) in enumerate(sets) if bi == 1] if nb == 2 else []
        p1 = [si for si in range(len(sets)) if si not in p2]
        tsc2 = self.fw.scope()
        tsc2.__enter__()
        self.ms = dict(l=l, sets=sets, tsc2=tsc2, p1=p1, p2=p2, tk_bufs={}, routed=set(), started=set())
        for si in p2:
            self.moe_alloc(si, tsc2)
        tsc1 = self.fw.scope()
        tsc1.__enter__()
        self.ms["tsc1"] = tsc1
        for si in p1:
            self.moe_alloc(si, tsc1)

    def moe_alloc(self, si, tsc=None):
        ms = self.ms
        if si in ms["tk_bufs"]:
            return
        bi, is_ctx = ms["sets"][si]
        ntok = NCX if is_ctx else NL
        cap = 2 * ntok // NE
        ms["tk_bufs"][si] = ([tsc.sb("affT", [NE, ntok], F32) for _ in range(2)], tsc.sb("vals", [NE, cap], F32),
                             tsc.sb("idxs", [NE, cap], U32), cap)

    def moe_route(self, si):
        fw = self.fw
        ms = self.ms
        l = ms["l"]
        bi, is_ctx = ms["sets"][si]
        self.moe_alloc(si)
        ms["routed"].add(si)
        ntok = NCX if is_ctx else NL
        cap = 2 * ntok // NE
        r = 2 if is_ctx else bi
        affT = ms["tk_bufs"][si][0]
        with fw.scope() as sc:
            W = self.norm_work(sc)
            arow, b_arow = self.make_arow(sc, l, r, "sc2", self.norm_ffn[l:l + 1, :])
            srow, b_srow = self.load_row(sc, "srow", self.mod_row(l, r, "sh2"))
            wr, b_wr = sc.sb("wr", [128, 8, NE], F32)
            fw.dma("sp", lambda e: e.dma_start(
                out=wr[:], in_=self.moe_router[l, :, :].rearrange("(k p) n -> p k n", p=128)), writes=[b_wr])
            h32s = [sc.sb("h32", [128, D], F32) for _ in range(2)]
            h16s = [sc.sb("h16", [128, D], BF16) for _ in range(2)]
            hT, b_hT = sc.sb("hT32", [128, 8, 128], F32)
            tps = [sc.ps("tps", [128, 4, 128], F32) for _ in range(2)]
            lps, b_lps = sc.ps("lps", [128, NE], F32)
            aps, b_aps = sc.ps("aps", [NE, 128], F32)
            sm, b_sm = sc.sb("sm", [128, 4], F32)
            ex, b_ex = sc.sb("ex", [128, NE], F32)
            aff, b_aff = sc.sb("aff", [128, NE], F32)

            def src_of(t):
                return (self.xcs[bi][t * 128:(t + 1) * 128, :], self.b_xcs[bi]) if is_ctx else \
                       (self.outs[bi][t * 128:(t + 1) * 128, :], self.b_outs[bi])
            ntile = ntok // 128
            nxt_x = self.norm_load(W, *src_of(0))
            for t in range(ntile):
                cur_x = nxt_x
                if t + 1 < ntile:
                    nxt_x = self.norm_load(W, *src_of(t + 1))
                h32, b_h32 = h32s[t % 2]
                h16, b_h16 = h16s[t % 2]
                self.norm_tile(W, cur_x, arow, b_arow, srow, b_srow, h32[:], b_h32)
                fw.op("act", lambda e: e.activation(out=h16[:], in_=h32[:], func=AF.Copy), reads=[b_h32], writes=[b_h16])
                dst = self.h2c[bi][t * 128:(t + 1) * 128, :] if is_ctx else self.h2l[bi][t * 128:(t + 1) * 128, :]
                fw.dma("sp", lambda e: e.dma_start(out=dst, in_=h16[:]), reads=[b_h16], writes=[self.b_h2], waw=False)
                for half in range(2):
                    tp, b_tp = tps[half]
                    for k in range(4):
                        kk = half * 4 + k
                        fw.op("pe", lambda e: e.transpose(tp[:, k, :], h32[:, kk * 128:(kk + 1) * 128], self.ident_f[:]),
                              reads=[b_h32, self.b_ident], writes=[b_tp])
                    fw.op("dve", lambda e: e.tensor_copy(out=hT[:, half * 4:(half + 1) * 4, :], in_=tp[:]),
                          reads=[b_tp], writes=[b_hT])
                for k in range(8):
                    fw.op("pe", lambda e: e.matmul(lps[:], lhsT=hT[:, k, :], rhs=wr[:, k, :], start=(k == 0), stop=(k == 7)),
                          reads=[b_hT, b_wr], writes=[b_lps])
                fw.op("dve", lambda e: e.tensor_reduce(out=sm[:, 0:1], in_=lps[:], axis=AX.X, op=ALU.max, negate=True),
                      reads=[b_lps], writes=[b_sm])
                fw.op("act", lambda e: e.activation(out=ex[:], in_=lps[:], func=AF.Exp, bias=sm[:, 0:1], accum_out=sm[:, 1:2]),
                      reads=[b_lps, b_sm], writes=[b_ex, b_sm])
                fw.op("dve", lambda e: e.reciprocal(out=sm[:, 2:3], in_=sm[:, 1:2]), reads=[b_sm], writes=[b_sm])
                fw.op("dve", lambda e: e.tensor_scalar(out=aff[:], in0=ex[:], scalar1=sm[:, 2:3], scalar2=None, op0=ALU.mult),
                      reads=[b_ex, b_sm], writes=[b_aff])
                fw.op("pe", lambda e: e.transpose(aps[:], aff[:], self.ident_f[:]), reads=[b_aff, self.b_ident], writes=[b_aps])
                fw.op("dve", lambda e: e.tensor_copy(out=affT[0][0][:, t * 128:(t + 1) * 128], in_=aps[:]),
                      reads=[b_aps], writes=[affT[0][1]])

    def moe_topk_gen(self, sis):
        fw = self.fw
        tk_bufs = self.ms["tk_bufs"]
        for si in sis:
            self.ms["started"].add(si)
        max_rounds = max(tk_bufs[si][3] // 8 for si in sis)
        for rd in range(max_rounds):
            for si in sis:
                affT, (vals, b_vals), (idxs, b_idxs), cap = tk_bufs[si]
                if rd >= cap // 8:
                    continue
                cur, b_cur = affT[rd % 2]
                nxt, b_nxt = affT[(rd + 1) % 2]
                v8 = vals[:, rd * 8:(rd + 1) * 8]
                fw.op("dve", lambda e: e.max(out=v8, in_=cur[:]), reads=[b_cur], writes=[b_vals])
                fw.op("dve", lambda e: e.max_index(out=idxs[:, rd * 8:(rd + 1) * 8], in_max=v8, in_values=cur[:]),
                      reads=[b_cur, b_vals], writes=[b_idxs])
                if rd < cap // 8 - 1:
                    fw.op("dve", lambda e: e.match_replace(out=nxt[:], in_to_replace=v8, in_values=cur[:], imm_value=-1.0),
                          reads=[b_cur, b_vals], writes=[b_nxt])
                yield

    def bg_step(self, n):
        for _ in range(n):
            if self.bg is None:
                return
            try:
                next(self.bg)
            except StopIteration:
                self.bg = None

    def bg_drain(self):
        while self.bg is not None:
            self.bg_step(16)

    def phase_moe(self, l):
        fw = self.fw
        nb = self.nb
        last = l == DEPTH - 1
        if self.ms is None:
            self.moe_begin(l)
        ms = self.ms
        sets = ms["sets"]
        p1, p2 = ms["p1"], ms["p2"]
        tk_bufs = ms["tk_bufs"]
        self.bg_drain()
        for si in range(len(sets)):
            if si not in ms["routed"]:
                self.moe_route(si)
        fg = [si for si in p1 if si not in ms["started"]]
        if fg:
            for _ in self.moe_topk_gen(fg):
                pass

        def store_tk(si):
            affT, (vals, b_vals), (idxs, b_idxs), cap = tk_bufs[si]
            fw.dma("sp", lambda e: e.dma_start(out=self.tkv[si, :, 0:cap], in_=vals[:]), reads=[b_vals], writes=[self.b_tk], waw=False)
            fw.dma("sp", lambda e: e.dma_start(out=self.tki[si, :, 0:cap], in_=idxs[:]), reads=[b_idxs], writes=[self.b_tk], waw=False)
        for si in p1:
            store_tk(si)
        ms["tsc1"].__exit__(None, None, None)
        if p2:
            self.bg = self.moe_topk_gen(p2)
        bgc = [0]

        def bg_slot():
            bgc[0] += 1
            if bgc[0] % 2 == 0:
                self.bg_step(1)
        with fw.scope() as sc:
            wbufs = [[sc.sb(f"w{m}", [128, 8, D], BF16) for m in range(3)] for _ in range(2)]
            grows = {}
            for bi in range(nb):
                grows[(bi, 0)] = self.load_row(sc, "g2row", self.mod_row(l, bi, "g2"))
            if not last:
                g = self.load_row(sc, "g2rowc", self.mod_row(l, 2, "g2"))
                for bi in range(nb):
                    grows[(bi, 1)] = g
            sl = {}

            def load_slots(si):
                bi, is_ctx = sets[si]
                ntok = NCX if is_ctx else NL
                cap = 2 * ntok // NE
                nblk = max(1, cap // 128)
                rows = min(cap, 128)
                ix, b_ix = sc.sb("ix", [128, NE, nblk], U32)
                gv, b_gv = sc.sb("gv", [128, NE, nblk], F32)
                fw.dma("sp", lambda e: e.dma_start(out=ix[0:rows], in_=self.tki[si, :, 0:cap].rearrange("e (p k) -> p e k", k=nblk),
                                                   allow_slow_non_contiguous=True), reads=[self.b_tk], writes=[b_ix])
                fw.dma("sp", lambda e: e.dma_start(out=gv[0:rows], in_=self.tkv[si, :, 0:cap].rearrange("e (p k) -> p e k", k=nblk),
                                                   allow_slow_non_contiguous=True), reads=[self.b_tk], writes=[b_gv])
                sl[si] = (ix, b_ix, gv, b_gv, nblk, rows)
            for si in p1:
                load_slots(si)
            xgs = [sc.sb("xg", [128, D], BF16) for _ in range(8)]
            xgT = [sc.sb("xgT", [128, 8, 512], BF16) for _ in range(2)]
            actT = [sc.sb("actT", [128, 8, 512], BF16) for _ in range(2)]
            sas = [sc.sb("sa", [128, 512], F32) for _ in range(1)]
            yscs = [sc.sb("ysc", [128, D], F32) for _ in range(2)]
            tpp = [sc.ps("tpp", [128, 8, 128], BF16) for _ in range(2)]
            a_ps = [sc.ps("a_ps", [128, 512]) for _ in range(2)]
            u_ps = [sc.ps("u_ps", [128, 512]) for _ in range(2)]
            y_ps = [sc.ps("y_ps", [128, 512]) for _ in range(2)]
            cnt = dict(xg=0, tp=0, au=0, y=0, ysc=0)
            wsrc = (self.moe_w_gate, self.moe_w_up, self.moe_w_down)

            def load_w(wk):
                wb = wbufs[wk % 2]
                ex_i = wk % NE
                for m in range(3):
                    for hh in range(2):
                        fw.dma("poolw", lambda e: e.dma_start(
                            out=wb[m][0][:, hh * 4:(hh + 1) * 4, :],
                            in_=wsrc[m][l, ex_i, hh * 512:(hh + 1) * 512, :].rearrange("(k p) n -> p k n", p=128)),
                            writes=[wb[m][1]], waw=False)

            items = [(ex_i, ex_i, si) for ex_i in range(NE) for si in p1] + \
                    [(NE + ex_i, ex_i, si) for ex_i in range(NE) for si in p2]
            n1 = NE * len(p1)
            nW = NE * (2 if p2 else 1)
            trans_done = [not p2]

            def ensure_slots(j):
                if j >= n1 and not trans_done[0]:
                    trans_done[0] = True
                    self.bg_drain()
                    for si in p2:
                        store_tk(si)
                        load_slots(si)

            xg_of = {}

            def stage_gather(j):
                ensure_slots(j)
                wk, ex_i, si = items[j]
                bi, is_ctx = sets[si]
                ix, b_ix, gv, b_gv, nblk, rows = sl[si]
                src = self.h2c[bi][:, :] if is_ctx else self.h2l[bi][:, :]
                xg_of[j] = []
                for k in range(nblk):
                    xg, b_xg = xgs[cnt["xg"] % len(xgs)]
                    cnt["xg"] += 1
                    xg_of[j].append((xg, b_xg))
                    fw.dma("pool", lambda e: e.indirect_dma_start(
                        out=xg[0:rows, :], out_offset=None, in_=src,
                        in_offset=bass.IndirectOffsetOnAxis(ap=ix[0:rows, ex_i, k:k + 1], axis=0)),
                        reads=[b_ix, self.b_h2], writes=[b_xg])

            def stage_tr(j):
                wk, ex_i, si = items[j]
                ix, b_ix, gv, b_gv, nblk, rows = sl[si]
                xT, b_xT = xgT[j % 2]
                for k in range(nblk):
                    xg, b_xg = xg_of[j][k]
                    tp, b_tp = tpp[cnt["tp"] % 2]
                    cnt["tp"] += 1
                    for kk in range(8):
                        fw.op("pe", lambda e: e.transpose(tp[:, kk, 0:rows], xg[0:rows, kk * 128:(kk + 1) * 128],
                                                          self.ident_bf[0:rows, 0:rows]),
                              reads=[b_xg, self.b_ident], writes=[b_tp])
                    fw.op("act", lambda e: e.activation(out=xT[:, :, k * rows:(k + 1) * rows], in_=tp[:, :, 0:rows], func=AF.Copy),
                          reads=[b_tp], writes=[b_xT])

            def stage_ffn(j):
                wk, ex_i, si = items[j]
                bi, is_ctx = sets[si]
                ix, b_ix, gv, b_gv, nblk, rows = sl[si]
                ncol = nblk * rows
                (wg, b_wg), (wu, b_wu), (wd, b_wd) = wbufs[wk % 2]
                dst, b_dst = (self.xcs[bi][:, :], self.b_xcs[bi]) if is_ctx else (self.outs[bi][:, :], self.b_outs[bi])
                grow, b_grow = grows[(bi, is_ctx)]
                xT, b_xT = xgT[j % 2]
                aT, b_aT = actT[j % 2]
                for fc in range(8):
                    ap_, b_ap = a_ps[cnt["au"] % 2]
                    up_, b_up = u_ps[cnt["au"] % 2]
                    sa, b_sa = sas[0]
                    cnt["au"] += 1
                    for kk in range(8):
                        fw.op("pe", lambda e: e.matmul(ap_[:, 0:ncol], lhsT=wg[:, kk, fc * 128:(fc + 1) * 128], rhs=xT[:, kk, 0:ncol],
                                                       start=(kk == 0), stop=(kk == 7)), reads=[b_wg, b_xT], writes=[b_ap])
                    for kk in range(8):
                        fw.op("pe", lambda e: e.matmul(up_[:, 0:ncol], lhsT=wu[:, kk, fc * 128:(fc + 1) * 128], rhs=xT[:, kk, 0:ncol],
                                                       start=(kk == 0), stop=(kk == 7)), reads=[b_wu, b_xT], writes=[b_up])
                    fw.op("act", lambda e: e.activation(out=sa[:, 0:ncol], in_=ap_[:, 0:ncol], func=AF.Silu), reads=[b_ap], writes=[b_sa])
                    fw.op("dve", lambda e: e.tensor_tensor(out=aT[:, fc, 0:ncol], in0=sa[:, 0:ncol], in1=up_[:, 0:ncol], op=ALU.mult),
                          reads=[b_sa, b_up], writes=[b_aT])
                    bg_slot()
                for k in range(nblk):
                    ysc, b_ysc = yscs[cnt["ysc"] % 2]
                    cnt["ysc"] += 1
                    for half in range(2):
                        yp, b_yp = y_ps[cnt["y"] % 2]
                        cnt["y"] += 1
                        for fc in range(8):
                            fw.op("pe", lambda e: e.matmul(yp[0:rows, :], lhsT=aT[:, fc, k * rows:(k + 1) * rows],
                                                           rhs=wd[:, fc, half * 512:(half + 1) * 512],
                                                           start=(fc == 0), stop=(fc == 7)), reads=[b_aT, b_wd], writes=[b_yp])
                        fw.op("dve", lambda e: e.scalar_tensor_tensor(
                            out=ysc[0:rows, half * 512:(half + 1) * 512], in0=yp[0:rows, :], scalar=gv[0:rows, ex_i, k:k + 1],
                            in1=grow[0:rows, half * 512:(half + 1) * 512], op0=ALU.mult, op1=ALU.mult),
                            reads=[b_yp, b_gv, b_grow], writes=[b_ysc])
                        bg_slot()
                    fw.dma("pool", lambda e: e.indirect_dma_start(
                        out=dst, out_offset=bass.IndirectOffsetOnAxis(ap=ix[0:rows, ex_i, k:k + 1], axis=0),
                        in_=ysc[0:rows, :], in_offset=None, compute_op=ALU.add),
                        reads=[b_ysc, b_ix], writes=[b_dst], waw=(k == 0))

            load_w(0)
            stage_gather(0)
            if len(items) > 1:
                stage_gather(1)
            stage_tr(0)
            for j, (wk, ex_i, si) in enumerate(items):
                if (j == 0 or items[j - 1][0] != wk) and wk + 1 < nW:
                    load_w(wk + 1)
                if j + 2 < len(items):
                    stage_gather(j + 2)
                if j + 1 < len(items):
                    stage_tr(j + 1)
                stage_ffn(j)
            ensure_slots(len(items))
        self.bg_drain()
        ms["tsc2"].__exit__(None, None, None)
        self.ms = None

    def attn_consts(self, sc):
        fw = self.fw
        bd32, b_bd32 = sc.sb("bd32", [128, 128], F32)
        r32, b_r32 = sc.sb("r32", [128, 128], F32)
        r32b, b_r32b = sc.sb("r32b", [128, 128], F32)
        bd, b_bd = sc.sb("bd", [128, 128], BF16)
        rb, b_rb = sc.sb("rblk", [128, 128], BF16)
        sel, b_sel = sc.sb("sel", [65, 64], F32)
        fw.op("pool", lambda e: e.memset(bd32[:], 0.0), writes=[b_bd32])
        fw.op("pool", lambda e: e.memset(bd32[0:64, 0:64], 1.0), writes=[b_bd32])
        fw.op("pool", lambda e: e.memset(bd32[64:128, 64:128], 1.0), writes=[b_bd32])
        fw.op("pool", lambda e: e.tensor_copy(out=bd[:], in_=bd32[:]), reads=[b_bd32], writes=[b_bd])
        fw.op("pool", lambda e: e.memset(r32[:], -1.0), writes=[b_r32])
        fw.op("pool", lambda e: e.affine_select(out=r32[:], in_=r32[:], pattern=[[-1, 128]], compare_op=ALU.is_equal,
                                                 fill=0.0, base=-32, channel_multiplier=1), reads=[b_r32], writes=[b_r32])
        fw.op("pool", lambda e: e.memset(r32b[:], 1.0), writes=[b_r32b])
        fw.op("pool", lambda e: e.affine_select(out=r32b[:], in_=r32b[:], pattern=[[-1, 128]], compare_op=ALU.is_equal,
                                                 fill=0.0, base=32, channel_multiplier=1), reads=[b_r32b], writes=[b_r32b])
        fw.op("pool", lambda e: e.tensor_tensor(out=r32[:], in0=r32[:], in1=r32b[:], op=ALU.add), reads=[b_r32, b_r32b], writes=[b_r32])
        fw.op("pool", lambda e: e.tensor_tensor(out=r32[:], in0=r32[:], in1=bd32[:], op=ALU.mult), reads=[b_r32, b_bd32], writes=[b_r32])
        fw.op("pool", lambda e: e.tensor_copy(out=rb[:], in_=r32[:]), reads=[b_r32], writes=[b_rb])
        fw.op("pool", lambda e: e.memset(sel[:], 0.0), writes=[b_sel])
        fw.op("pool", lambda e: e.memset(sel[64:65, :], 1.0), writes=[b_sel])
        return (bd, b_bd), (rb, b_rb), (sel, b_sel)

    def phase_attn(self, l, bi):
        fw = self.fw
        j = l // 2
        last = l == DEPTH - 1
        qTd = self.nc.dram_tensor(f"qTd_{l}_{bi}", [128, 8, NT], BF16).ap()
        b_qTd = Buf()
        with fw.scope() as osc:
            kT, b_kT = osc.sb("kT", [128, 2, NT], BF16)
            vA, b_vA = osc.sb("vA", [128, NT // 128, 4, 128], BF16)
            negC, b_negC = osc.sb("negC", [128, 1], F32)
            (bd, b_bd), (rb, b_rb), (sel, b_sel) = self.attn_consts(osc)
            fw.op("pool", lambda e: e.memset(vA[:, :, :, 64:128], 1.0), writes=[b_vA])
            with fw.scope() as sc:
                W = self.norm_work(sc)
                win, b_win = sc.sb("win", [128, 8, 1536], BF16)
                for kk in range(8):
                    for a_ in range(2):
                        for two in range(2):
                            c0 = a_ * 512 + two * 256
                            fw.dma("pool", lambda e: e.dma_start(
                                out=win[:, kk, a_ * 512:(a_ + 1) * 512].rearrange("p (r two d) -> p r two d", r=4, two=2, d=64)[:, :, two, :],
                                in_=self.attn_w_in[j, kk * 128:(kk + 1) * 128, c0:c0 + 256].rearrange("p (r d) -> p r d", r=4, d=64)),
                                writes=[b_win], waw=False)
                for hh in range(2):
                    fw.dma("pool", lambda e: e.dma_start(
                        out=win[:, hh * 4:(hh + 1) * 4, 1024:1536],
                        in_=self.attn_w_in[j, hh * 512:(hh + 1) * 512, 1024:1536].rearrange("(k p) n -> p k n", p=128)),
                        writes=[b_win], waw=False)
                rows = {}
                for r in ((bi, 2) if True else ()):
                    rows[r] = (self.make_arow(sc, l, r, "sc1", self.norm_mix[l:l + 1, :]),
                               self.load_row(sc, "srow", self.mod_row(l, r, "sh1")))
                gq, b_gq = sc.sb("gq", [128, 1], F32)
                gk, b_gk = sc.sb("gk", [128, 1], F32)
                for hf in range(2):
                    fw.dma("sp", lambda e: e.dma_start(out=gq[hf * 64:(hf + 1) * 64, :],
                                                       in_=self.attn_q_norm[j, :].rearrange("(p o) -> p o", o=1)), writes=[b_gq])
                    fw.dma("sp", lambda e: e.dma_start(out=gk[hf * 64:(hf + 1) * 64, :],
                                                       in_=self.attn_k_norm[j, :].rearrange("(p o) -> p o", o=1)), writes=[b_gk])
                gr, b_gr = sc.sb("gr", [128, 2, 64], F32)
                mx, b_mx = sc.sb("mx", [128, 2], F32)
                fw.dma("sp", lambda e: e.dma_start(out=gr[:, 0, :], in_=self.attn_q_norm[j:j + 1, :].partition_broadcast(128)), writes=[b_gr])
                fw.dma("sp", lambda e: e.dma_start(out=gr[:, 1, :], in_=self.attn_k_norm[j:j + 1, :].partition_broadcast(128)), writes=[b_gr])
                fw.op("dve", lambda e: e.tensor_reduce(out=mx[:], in_=gr[:], axis=AX.X, op=ALU.max, apply_absolute_value=True),
                      reads=[b_gr], writes=[b_mx])
                fw.op("dve", lambda e: e.scalar_tensor_tensor(out=negC[:], in0=mx[:, 0:1], scalar=-8.0, in1=mx[:, 1:2],
                                                              op0=ALU.mult, op1=ALU.mult), reads=[b_mx], writes=[b_negC])
                h16s = [sc.sb("h16", [128, D], BF16) for _ in range(2)]
                hTs = [sc.sb("hT", [128, 8, 512], BF16) for _ in range(2)]
                qst = [sc.sb("qst", [128, 8, 512], BF16) for _ in range(1)]
                cs = [sc.sb("cos", [128, 512], F32) for _ in range(1)]
                sn = [sc.sb("sin", [128, 512], F32) for _ in range(1)]
                sqs = [sc.sb("sq", [128, 512], BF16) for _ in range(2)]
                qgs = [sc.sb("qg", [128, 512], BF16) for _ in range(2)]
                lnv, b_lnv = sc.sb("lnv", [128, 512], F32)
                rstds = [sc.sb("rstd", [128, 512], F32) for _ in range(2)]
                t1s = [sc.sb("t1", [128, 512], F32) for _ in range(2)]
                t2s = [sc.sb("t2", [128, 512], F32) for _ in range(1)]
                tpp = [sc.ps("tpp", [128, 8, 128], BF16) for _ in range(2)]
                q_ps = [sc.ps("q_ps", [128, 512]) for _ in range(2)]
                ss_ps, b_ss = sc.ps("ss_ps", [128, 512])
                rq_ps, b_rq = sc.ps("rq_ps", [128, 512])
                v_ps = [sc.ps("v_ps", [128, 256]) for _ in range(2)]
                cnt = dict(t=0, c=0, v=0)
                groups = [(0, 2)] + [(2 + 4 * g, 4) for g in range(8)]
                tiles_all = [t for t0, n in groups for t in range(t0, t0 + n)]
                nxt_x = self.norm_load(W, *self.res_tile(bi, 0))
                ti = 0
                for gi, (t0, nt) in enumerate(groups):
                    is_ctx = gi == 0
                    ntg = nt * 128
                    u0 = t0 * 128
                    (arow, b_arow), (srow, b_srow) = rows[2 if is_ctx else bi]
                    hT, b_hT = hTs[gi % 2]
                    qs, b_qs = qst[0]
                    if not is_ctx:
                        cg, b_cg = cs[0]
                        sg, b_sg = sn[0]
                        l0 = u0 - NCX
                        fw.dma("sp", lambda e: e.dma_start(out=cg[:], in_=self.rope_cos[:, l0:l0 + 512]), writes=[b_cg])
                        fw.dma("sp", lambda e: e.dma_start(out=sg[:], in_=self.rope_sin[:, l0:l0 + 512]), writes=[b_sg])
                    for tt in range(nt):
                        cur_x = nxt_x
                        ti += 1
                        if ti < len(tiles_all):
                            nxt_x = self.norm_load(W, *self.res_tile(bi, tiles_all[ti]))
                        h16, b_h16 = h16s[cnt["t"] % 2]
                        tp, b_tp = tpp[cnt["t"] % 2]
                        cnt["t"] += 1
                        self.norm_tile(W, cur_x, arow, b_arow, srow, b_srow, h16[:], b_h16)
                        for kk in range(8):
                            fw.op("pe", lambda e: e.transpose(tp[:, kk, :], h16[:, kk * 128:(kk + 1) * 128], self.ident_bf[:]),
                                  reads=[b_h16, self.b_ident], writes=[b_tp])
                        fw.op("act", lambda e: e.activation(out=hT[:, :, tt * 128:(tt + 1) * 128], in_=tp[:], func=AF.Copy),
                              reads=[b_tp], writes=[b_hT])
                        vp, b_vp = v_ps[cnt["v"] % 2]
                        cnt["v"] += 1
                        for kk in range(8):
                            fw.op("pe", lambda e: e.matmul(vp[:], lhsT=hT[:, kk, tt * 128:(tt + 1) * 128], rhs=win[:, kk, 1280:1536],
                                                           start=(kk == 0), stop=(kk == 7)), reads=[b_hT, b_win], writes=[b_vp])
                        fw.op("dve", lambda e: e.tensor_copy(out=vA[:, t0 + tt, :, 0:64], in_=vp[:].rearrange("p (g d) -> p g d", g=4)),
                              reads=[b_vp], writes=[b_vA])
                        self.bg_step(2)
                    for c in range(10):
                        qp, b_qp = q_ps[cnt["c"] % 2]
                        sq, b_sq = sqs[cnt["c"] % 2]
                        qg, b_qg = qgs[cnt["c"] % 2]
                        rstd, b_rstd = rstds[cnt["c"] % 2]
                        t1, b_t1 = t1s[cnt["c"] % 2]
                        t2, b_t2 = t2s[0]
                        cnt["c"] += 1
                        is_q = c < 8
                        for kk in range(8):
                            lw = win[:, kk, c * 128:(c + 1) * 128]
                            fw.op("pe", lambda e: e.matmul(qp[:, 0:ntg], lhsT=lw, rhs=hT[:, kk, 0:ntg], start=(kk == 0), stop=(kk == 7)),
                                  reads=[b_win, b_hT], writes=[b_qp])
                        gcol, b_gcol = (gq, b_gq) if is_q else (gk, b_gk)
                        fw.op("act", lambda e: e.activation(out=sq[:, 0:ntg], in_=qp[:, 0:ntg], func=AF.Square), reads=[b_qp], writes=[b_sq])
                        fw.op("act", lambda e: e.activation(out=qg[:, 0:ntg], in_=qp[:, 0:ntg], func=AF.Copy, scale=gcol[:, 0:1]),
                              reads=[b_qp, b_gcol], writes=[b_qg])
                        fw.op("pe", lambda e: e.matmul(ss_ps[:, 0:ntg], lhsT=bd[:], rhs=sq[:, 0:ntg], start=True, stop=True),
                              reads=[b_bd, b_sq], writes=[b_ss])
                        if not is_ctx:
                            fw.op("pe", lambda e: e.matmul(rq_ps[:, 0:ntg], lhsT=rb[:], rhs=qg[:, 0:ntg], start=True, stop=True),
                                  reads=[b_rb, b_qg], writes=[b_rq])
                        fw.op("act", lambda e: e.activation(out=lnv[:, 0:ntg], in_=ss_ps[:, 0:ntg], func=AF.Ln, scale=1.0 / 64, bias=EPS),
                              reads=[b_ss], writes=[b_lnv])
                        fw.op("act", lambda e: e.activation(out=rstd[:, 0:ntg], in_=lnv[:, 0:ntg], func=AF.Exp, scale=-0.5),
                              reads=[b_lnv], writes=[b_rstd])
                        if is_q:
                            dst, b_dst = qs[:, c, 0:ntg], b_qs
                        else:
                            dst, b_dst = kT[:, c - 8, u0:u0 + ntg], b_kT
                        if is_ctx:
                            fw.op("dve", lambda e: e.tensor_tensor(out=dst, in0=qg[:, 0:ntg], in1=rstd[:, 0:ntg], op=ALU.mult),
                                  reads=[b_qg, b_rstd], writes=[b_dst])
                        else:
                            fw.op("dve", lambda e: e.tensor_tensor(out=t1[:], in0=qg[:], in1=cg[:], op=ALU.mult),
                                  reads=[b_qg, b_cg], writes=[b_t1])
                            fw.op("dve", lambda e: e.tensor_tensor(out=t2[:], in0=rq_ps[:], in1=sg[:], op=ALU.mult),
                                  reads=[b_rq, b_sg], writes=[b_t2])
                            fw.op("pool", lambda e: e.tensor_tensor(out=t1[:], in0=t1[:], in1=t2[:], op=ALU.add),
                                  reads=[b_t1, b_t2], writes=[b_t1])
                            fw.op("dve", lambda e: e.tensor_tensor(out=dst, in0=t1[:], in1=rstd[:], op=ALU.mult),
                                  reads=[b_t1, b_rstd], writes=[b_dst])
                    fw.dma("sp", lambda e: e.dma_start(out=qTd[:, :, u0:u0 + ntg], in_=qs[:, :, 0:ntg]), reads=[b_qs], writes=[b_qTd], waw=False)
            with fw.scope() as sc:
                wo, b_wo = sc.sb("wo", [64, 16, D], BF16)
                for hh in range(2):
                    fw.dma("pool", lambda e: e.dma_start(
                        out=wo[:, hh * 8:(hh + 1) * 8, :],
                        in_=self.attn_w_out[j, hh * 512:(hh + 1) * 512, :].rearrange("(h p) n -> p h n", p=64)),
                        writes=[b_wo], waw=False)
                g1 = {bi: self.load_row(sc, "g1row", self.mod_row(l, bi, "g1"))}
                if not last:
                    g1[2] = self.load_row(sc, "g1rowc", self.mod_row(l, 2, "g1"))
                qts = [sc.sb("qt", [128, 16, 128], BF16) for _ in range(3)]
                for qt_, b_qt_ in qts:
                    fw.op("pool", lambda e: e.memset(qt_[:], 0.0), writes=[b_qt_])
                pTs = [sc.sb("pT", [128, 1024], BF16) for _ in range(4)]
                osbs = [sc.sb("osb", [65, 512], F32) for _ in range(2)]
                rdens = [sc.sb("rden", [64, 512], F32) for _ in range(2)]
                oTns = [sc.sb("oTn", [64, 4, 512], BF16) for _ in range(2)]
                xts = [sc.sb("xres", [128, D], F32) for _ in range(3)]
                tmps = [sc.sb("xtmp", [128, D], F32) for _ in range(2)]
                s_ps = [sc.ps("s_ps", [128, 1024]) for _ in range(2)]
                o_ps = [sc.ps("o_ps", [128, 512]) for _ in range(2)]
                den_ps, b_den = sc.ps("den_ps", [64, 512])
                y_ps, b_yp = sc.ps("y_ps", [128, 512])
                qtiles = ([] if last else [0, 1]) + list(range(2, NT // 128))
                LOOK = 2
                units = []
                for qi, t in enumerate(qtiles):
                    npair = (2 if t < 2 else NT // 128) // 2
                    for g in range(4):
                        for kp in range(npair):
                            units.append((qi, t, g, kp, npair))
                qstate = {}

                def q_begin(qi, t):
                    qt, b_qt = qts[qi % 3]
                    xt, b_xt = xts[qi % 3]
                    for g_ in range(4):
                        hf_, c0_ = g_ % 2, (g_ // 2) * 4
                        fw.dma("sp", lambda e: e.dma_start(out=qt[hf_ * 64:(hf_ + 1) * 64, g_ * 4:(g_ + 1) * 4, :],
                                                           in_=qTd[hf_ * 64:(hf_ + 1) * 64, c0_:c0_ + 4, t * 128:(t + 1) * 128]),
                               reads=[b_qTd], writes=[b_qt])
                    rap, b_rap = self.res_tile(bi, t)
                    fw.dma("sp", lambda e: e.dma_start(out=xt[:], in_=rap), reads=[b_rap], writes=[b_xt])
                    qstate[qi] = (qt, b_qt, xt, b_xt, rap, b_rap)

                def issue_qk(ui):
                    qi, t, g, kp, npair = units[ui]
                    if qi not in qstate:
                        q_begin(qi, t)
                    qt, b_qt = qstate[qi][0:2]
                    hf, cq = g % 2, (g // 2) * 4
                    sp_, b_sp = s_ps[ui % 2]
                    pT, b_pT = pTs[ui % 4]
                    for i2 in range(2):
                        kb = kp * 2 + i2
                        fw.op("pe", lambda e: e.matmul(sp_[:, i2 * 512:(i2 + 1) * 512],
                                                       lhsT=kT[:, g // 2, kb * 128:(kb + 1) * 128],
                                                       rhs=qt[:, g * 4:(g + 1) * 4, :], start=True, stop=True),
                              reads=[b_kT, b_qt], writes=[b_sp])
                    fw.op("act", lambda e: e.activation(out=pT[:], in_=sp_[:], func=AF.Exp, scale=0.125, bias=negC[:, 0:1]),
                          reads=[b_sp, b_negC], writes=[b_pT])

                pending = []

                def fin1(qi, g, oi):
                    op_, b_op = o_ps[oi % 2]
                    osb, b_osb = osbs[oi % 2]
                    fw.op("dve", lambda e: e.tensor_copy(out=osb[:], in_=op_[0:65, :]), reads=[b_op], writes=[b_osb])

                def fin2(qi, g, oi):
                    osb, b_osb = osbs[oi % 2]
                    rden, b_rden = rdens[oi % 2]
                    oTn, b_oTn = oTns[qi % 2]
                    fw.op("pe", lambda e: e.matmul(den_ps[:], lhsT=sel[:], rhs=osb[:], start=True, stop=True),
                          reads=[b_sel, b_osb], writes=[b_den])
                    fw.op("dve", lambda e: e.reciprocal(out=rden[:], in_=den_ps[:]), reads=[b_den], writes=[b_rden])
                    fw.op("dve", lambda e: e.tensor_tensor(out=oTn[:, g, :], in0=osb[0:64, :], in1=rden[:], op=ALU.mult),
                          reads=[b_osb, b_rden], writes=[b_oTn])

                def fin3(qi, t):
                    qt, b_qt, xt, b_xt, rap, b_rap = qstate.pop(qi)
                    oTn, b_oTn = oTns[qi % 2]
                    tmp, b_tmp = tmps[qi % 2]
                    grow, b_grow = g1[2 if t < 2 else bi]
                    for half in range(2):
                        for h in range(16):
                            g, r_ = h // 4, h % 4
                            fw.op("pe", lambda e: e.matmul(y_ps[:], lhsT=oTn[:, g, r_ * 128:(r_ + 1) * 128],
                                                           rhs=wo[:, h, half * 512:(half + 1) * 512], start=(h == 0), stop=(h == 15)),
                                  reads=[b_oTn, b_wo], writes=[b_yp])
                        hs = slice(half * 512, (half + 1) * 512)
                        fw.op("dve", lambda e: e.tensor_tensor(out=tmp[:, hs], in0=y_ps[:], in1=grow[:, hs], op=ALU.mult),
                              reads=[b_yp, b_grow], writes=[b_tmp])
                        fw.op("pool", lambda e: e.tensor_tensor(out=tmp[:, hs], in0=tmp[:, hs], in1=xt[:, hs], op=ALU.add),
                              reads=[b_tmp, b_xt], writes=[b_tmp])
                    fw.dma("sp", lambda e: e.dma_start(out=rap, in_=tmp[:]), reads=[b_tmp], writes=[b_rap], waw=False)

                ocount = 0
                for ui in range(min(LOOK, len(units))):
                    issue_qk(ui)
                for ui, (qi, t, g, kp, npair) in enumerate(units):
                    if ui + LOOK < len(units):
                        issue_qk(ui + LOOK)
                    due = [p for p in pending if p[0] <= ui]
                    pending[:] = [p for p in pending if p[0] > ui]
                    for _, fn in due:
                        fn()
                    pT, b_pT = pTs[ui % 4]
                    op_, b_op = o_ps[ocount % 2]
                    for i2 in range(2):
                        kb = kp * 2 + i2
                        fw.op("pe", lambda e: e.matmul(op_[:, :], lhsT=vA[:, kb, g, :], rhs=pT[:, i2 * 512:(i2 + 1) * 512],
                                                       start=(kb == 0), stop=(kb == 2 * npair - 1)),
                              reads=[b_vA, b_pT], writes=[b_op])
                    if kp == npair - 1:
                        oi = ocount
                        ocount += 1
                        fin1(qi, g, oi)
                        pending.append((ui + 2, (lambda qi=qi, g=g, oi=oi: fin2(qi, g, oi))))
                        if g == 3:
                            pending.append((ui + 4, (lambda qi=qi, t=t: fin3(qi, t))))
                for _, fn in pending:
                    fn()

    def phase_mlstm(self, l, bi):
        fw = self.fw
        nc = self.nc
        j = l // 2
        NCH = NT // 64
        qk_d = nc.dram_tensor(f"qk_d_{l}_{bi}", [NCH, 128, 8, 64], BF16).ap()
        oT_d = nc.dram_tensor(f"oT_d_{l}_{bi}", [NT // 128, 128, 8, 128], BF16).ap()
        v_d = nc.dram_tensor(f"v_d_{l}_{bi}", [NT, D], BF16).ap()
        hf_d = nc.dram_tensor(f"hf_d_{l}_{bi}", [NT, D], F32).ap()
        b_qk, b_oT, b_v, b_hf = Buf(), Buf(), Buf(), Buf()
        order = {0: list(range(NCH)), 1: [3, 2, 1, 0] + list(range(NCH - 1, 3, -1))}
        with fw.scope() as osc:
            tokT, b_tokT = osc.sb("tokT", [64, NCH, 16], F32)
            WIbc, b_WIbc = osc.sb("WIbc", [128, 8, NCH], F32)
            with fw.scope() as gsc:
                IG, b_IG = gsc.sb("IG", [8, NT], F32)
                FG, b_FG = gsc.sb("FG", [8, NT], F32)
                with fw.scope() as sc:
                    W = self.norm_work(sc)
                    win, b_win = sc.sb("win", [128, 8, 3072], BF16)
                    wgp, b_wgp = sc.sb("wgp", [128, 8, 16], BF16)
                    for hh in range(2):
                        for cc in range(2):
                            fw.dma("pool", lambda e: e.dma_start(
                                out=win[:, hh * 4:(hh + 1) * 4, cc * 1536:(cc + 1) * 1536],
                                in_=self.mlstm_w_in[j, hh * 512:(hh + 1) * 512, cc * 1536:(cc + 1) * 1536].rearrange("(k p) n -> p k n", p=128)),
                                writes=[b_win], waw=False)
                    for dst0, src0 in ((0, 0), (4, 8), (8, 4), (12, 12)):
                        fw.dma("pool", lambda e: e.dma_start(
                            out=wgp[:, :, dst0:dst0 + 4],
                            in_=self.mlstm_w_in[j, :, 3072 + src0:3072 + src0 + 4].rearrange("(k p) n -> p k n", p=128)),
                            writes=[b_wgp], waw=False)
                    bg, b_bg = sc.sb("bg", [8, 2], F32)
                    for (prow, src0, col) in ((0, 0, 0), (4, 8, 0), (0, 4, 1), (4, 12, 1)):
                        fw.dma("sp", lambda e: e.dma_start(out=bg[prow:prow + 4, col:col + 1],
                                                           in_=self.mlstm_b_gate[j, src0:src0 + 4].rearrange("(p o) -> p o", o=1)),
                               writes=[b_bg])
                    fw.op("dve", lambda e: e.tensor_scalar(out=bg[:], in0=bg[:], scalar1=1.0 / 15.0, scalar2=None, op0=ALU.mult),
                          reads=[b_bg], writes=[b_bg])
                    nwrow = self.load_row(sc, "nwrow", self.norm_mix[l:l + 1, :])
                    rows = {}
                    for r in (bi, 2):
                        ar, b_ar = self.load_row(sc, "arow", self.mod_row(l, r, "sc1"))
                        fw.op("dve", lambda e: e.scalar_tensor_tensor(out=ar[:], in0=ar[:], scalar=1.0, in1=nwrow[0][:],
                                                                      op0=ALU.add, op1=ALU.mult), reads=[b_ar, nwrow[1]], writes=[b_ar])
                        rows[r] = ((ar, b_ar), self.load_row(sc, "srow", self.mod_row(l, r, "sh1")))
                    h16s = [sc.sb("h16", [128, D], BF16) for _ in range(2)]
                    hTs = [sc.sb("hT", [128, 8, 512], BF16) for _ in range(2)]
                    qkst, b_qkst = sc.sb("qkst", [128, 8, 512], BF16)
                    ost, b_ost = sc.sb("ost", [128, 8, 512], BF16)
                    vsts = [sc.sb("vst", [128, D], BF16) for _ in range(2)]
                    tpp = [sc.ps("tpp", [128, 8, 128], BF16) for _ in range(2)]
                    c_ps = [sc.ps("c_ps", [128, 512]) for _ in range(2)]
                    g_ps = [sc.ps("g_ps", [128, 512]) for _ in range(2)]
                    v_ps = [sc.ps("v_ps", [128, 512]) for _ in range(2)]
                    cnt = dict(t=0, c=0, v=0, vs=0)
                    groups = [(0, 2)] + [(2 + 4 * g, 4) for g in range(8)]
                    tiles_all = [t for t0, n in groups for t in range(t0, t0 + n)]
                    nxt_x = self.norm_load(W, *self.res_tile(bi, 0))
                    ti = 0
                    for gi, (t0, nt) in enumerate(groups):
                        is_ctx = gi == 0
                        ntg = nt * 128
                        u0 = t0 * 128
                        (arow, b_arow), (srow, b_srow) = rows[2 if is_ctx else bi]
                        hT, b_hT = hTs[gi % 2]
                        for tt in range(nt):
                            cur_x = nxt_x
                            ti += 1
                            if ti < len(tiles_all):
                                nxt_x = self.norm_load(W, *self.res_tile(bi, tiles_all[ti]))
                            h16, b_h16 = h16s[cnt["t"] % 2]
                            tp, b_tp = tpp[cnt["t"] % 2]
                            cnt["t"] += 1
                            self.norm_tile(W, cur_x, arow, b_arow, srow, b_srow, h16[:], b_h16)
                            for kk in range(8):
                                fw.op("pe", lambda e: e.transpose(tp[:, kk, :], h16[:, kk * 128:(kk + 1) * 128], self.ident_bf[:]),
                                      reads=[b_h16, self.b_ident], writes=[b_tp])
                            fw.op("act", lambda e: e.activation(out=hT[:, :, tt * 128:(tt + 1) * 128], in_=tp[:], func=AF.Copy),
                                  reads=[b_tp], writes=[b_hT])
                            vst, b_vst = vsts[cnt["vs"] % 2]
                            cnt["vs"] += 1
                            for half in range(2):
                                vp, b_vp = v_ps[cnt["v"] % 2]
                                cnt["v"] += 1
                                for kk in range(8):
                                    fw.op("pe", lambda e: e.matmul(vp[:], lhsT=hT[:, kk, tt * 128:(tt + 1) * 128],
                                                                   rhs=win[:, kk, 1024 + half * 512:1536 + half * 512],
                                                                   start=(kk == 0), stop=(kk == 7)), reads=[b_hT, b_win], writes=[b_vp])
                                fw.op("dve", lambda e: e.tensor_copy(out=vst[:, half * 512:(half + 1) * 512], in_=vp[:]),
                                      reads=[b_vp], writes=[b_vst])
                            ut = (t0 + tt) * 128
                            fw.dma("sp", lambda e: e.dma_start(out=v_d[ut:ut + 128, :], in_=vst[:]), reads=[b_vst], writes=[b_v], waw=False)
                            self.bg_step(2)
                        for c in range(16):
                            cp, b_cp = c_ps[cnt["c"] % 2]
                            cnt["c"] += 1
                            col0 = c * 128 if c < 8 else 2048 + (c - 8) * 128
                            for kk in range(8):
                                fw.op("pe", lambda e: e.matmul(cp[:, 0:ntg], lhsT=win[:, kk, col0:col0 + 128], rhs=hT[:, kk, 0:ntg],
                                                               start=(kk == 0), stop=(kk == 7)), reads=[b_win, b_hT], writes=[b_cp])
                            if c < 4:
                                fw.op("act", lambda e: e.activation(out=qkst[:, c, 0:ntg], in_=cp[:, 0:ntg], func=AF.Copy, scale=128 ** -0.5),
                                      reads=[b_cp], writes=[b_qkst])
                            elif c < 8:
                                fw.op("dve", lambda e: e.tensor_copy(out=qkst[:, c, 0:ntg], in_=cp[:, 0:ntg]), reads=[b_cp], writes=[b_qkst])
                            else:
                                fw.op("act", lambda e: e.activation(out=ost[:, c - 8, 0:ntg], in_=cp[:, 0:ntg], func=AF.Sigmoid),
                                      reads=[b_cp], writes=[b_ost])
                        for cc in range(ntg // 64):
                            ch = u0 // 64 + cc
                            fw.dma("sp", lambda e: e.dma_start(out=qk_d[ch], in_=qkst[:, :, cc * 64:(cc + 1) * 64]),
                                   reads=[b_qkst], writes=[b_qk], waw=False)
                        for tt in range(nt):
                            fw.dma("sp", lambda e: e.dma_start(out=oT_d[t0 + tt], in_=ost[:, :, tt * 128:(tt + 1) * 128]),
                                   reads=[b_ost], writes=[b_oT], waw=False)
                        for gt, (dst, b_dst) in enumerate(((IG, b_IG), (FG, b_FG))):
                            gp, b_gp = g_ps[gt]
                            for kk in range(8):
                                fw.op("pe", lambda e: e.matmul(gp[0:8, 0:ntg], lhsT=wgp[:, kk, gt * 8:(gt + 1) * 8], rhs=hT[:, kk, 0:ntg],
                                                               start=(kk == 0), stop=(kk == 7)), reads=[b_wgp, b_hT], writes=[b_gp])
                            fw.op("act", lambda e: e.activation(out=dst[:, u0:u0 + ntg], in_=gp[0:8, 0:ntg], func=AF.Tanh,
                                                                scale=1.0 / 15.0, bias=bg[:, gt:gt + 1]),
                                  reads=[b_gp, b_bg], writes=[b_dst])
                with fw.scope() as sc:
                    X2, b_X2 = sc.sb("X2", [8, NT], F32)
                    X3, b_X3 = sc.sb("X3", [8, NT], F32)
                    msk, b_msk = sc.sb("msk", [8, NT], F32)
                    dm, b_dm = sc.sb("dm", [8, 2], F32)
                    sm = {k: sc.sb(k, [8, NCH], F32) for k in ("amax", "blast", "mn0", "mn1", "mb0", "mb1", "mnext", "mbef", "Mp", "wi")}
                    rbd, b_rbd = sc.sb("rbd", [8, 8, NCH], F32)
                    ones8, b_ones8 = sc.sb("ones8", [8, 128], F32)
                    zero1, b_zero1 = sc.sb("zero1", [8, 1], F32)
                    tk_ps = [sc.ps("tk_ps", [64, 32, 16], F32) for _ in range(2)]
                    wi_ps = [sc.ps("wi_ps", [128, 512], F32) for _ in range(2)]
                    v3 = lambda t: t[:].rearrange("p (c s) -> p c s", s=64)
                    fw.op("pool", lambda e: e.memset(msk[:], 1.0), writes=[b_msk])
                    fw.op("pool", lambda e: e.memset(v3(msk)[:, :, 0:1], 0.0), writes=[b_msk])
                    fw.op("pool", lambda e: e.memset(ones8[:], 1.0), writes=[b_ones8])
                    fw.op("pool", lambda e: e.memset(zero1[:], 0.0), writes=[b_zero1])
                    fw.op("pool", lambda e: e.iota(dm[:, 0:1], pattern=[[0, 1]], base=0, channel_multiplier=1,
                                                   allow_small_or_imprecise_dtypes=True), writes=[b_dm])
                    fw.op("dve", lambda e: e.tensor_single_scalar(out=dm[:, 1:2], in_=dm[:, 0:1], scalar=3.5, op=ALU.is_ge),
                          reads=[b_dm], writes=[b_dm])
                    fw.op("act", lambda e: e.activation(out=FG[:], in_=FG[:], func=AF.Exp, scale=-15.0), reads=[b_FG], writes=[b_FG])
                    fw.op("act", lambda e: e.activation(out=FG[:], in_=FG[:], func=AF.Ln, bias=1.0), reads=[b_FG], writes=[b_FG])
                    fw.op("dve", lambda e: e.tensor_tensor_scan(out=X2[:], data0=msk[:], data1=FG[:], initial=0.0, op0=ALU.mult, op1=ALU.add),
                          reads=[b_msk, b_FG], writes=[b_X2])
                    tot = v3(X2)[:, :, 63:64]
                    fw.op("dve", lambda e: e.tensor_tensor(out=X3[:], in0=FG[:], in1=X2[:], op=ALU.subtract), reads=[b_FG, b_X2], writes=[b_X3])
                    fw.op("dve", lambda e: e.tensor_tensor(out=v3(X3), in0=v3(X3), in1=tot.to_broadcast([8, NCH, 64]), op=ALU.add),
                          reads=[b_X3, b_X2], writes=[b_X3])
                    fw.op("dve", lambda e: e.tensor_tensor(out=X3[:], in0=X3[:], in1=X2[:], op=ALU.subtract), reads=[b_X3, b_X2], writes=[b_X3])
                    fw.op("dve", lambda e: e.tensor_scalar(out=sm["blast"][0][:], in0=v3(X2)[:, :, 63], scalar1=-1.0, scalar2=None, op0=ALU.mult),
                          reads=[b_X2], writes=[sm["blast"][1]])
                    NB, b_NB = FG, b_FG
                    fw.op("dve", lambda e: e.scalar_tensor_tensor(out=NB[:], in0=X3[:], scalar=dm[:, 1:2], in1=X2[:], op0=ALU.mult, op1=ALU.add),
                          reads=[b_X3, b_dm, b_X2], writes=[b_NB])
                    fw.op("dve", lambda e: e.scalar_tensor_tensor(out=IG[:], in0=IG[:], scalar=15.0, in1=NB[:], op0=ALU.mult, op1=ALU.add),
                          reads=[b_IG, b_NB], writes=[b_IG])
                    amax, b_amax = sm["amax"]
                    blast, b_blast = sm["blast"]
                    fw.op("dve", lambda e: e.tensor_reduce(out=amax[:], in_=v3(IG), axis=AX.X, op=ALU.max), reads=[b_IG], writes=[b_amax])
                    for d in range(2):
                        mn, b_mn = sm[f"mn{d}"]
                        mb, b_mb = sm[f"mb{d}"]
                        prev = None
                        for c in order[d]:
                            sc_ap = zero1[:, 0:1] if prev is None else mn[:, prev:prev + 1]
                            fw.op("dve", lambda e: e.tensor_copy(out=mb[:, c:c + 1], in_=sc_ap), reads=[b_mn, b_zero1], writes=[b_mb])
                            fw.op("dve", lambda e: e.scalar_tensor_tensor(out=mn[:, c:c + 1], in0=amax[:, c:c + 1], scalar=sc_ap,
                                                                          in1=blast[:, c:c + 1], op0=ALU.max, op1=ALU.add),
                                  reads=[b_amax, b_blast, b_mn, b_zero1], writes=[b_mn])
                            prev = c
                    for nm, (a0, a1) in (("mnext", ("mn0", "mn1")), ("mbef", ("mb0", "mb1"))):
                        o_, b_o = sm[nm]
                        fw.op("dve", lambda e: e.tensor_tensor(out=o_[:], in0=sm[a1][0][:], in1=sm[a0][0][:], op=ALU.subtract),
                              reads=[sm[a0][1], sm[a1][1]], writes=[b_o])
                        fw.op("dve", lambda e: e.scalar_tensor_tensor(out=o_[:], in0=o_[:], scalar=dm[:, 1:2], in1=sm[a0][0][:],
                                                                      op0=ALU.mult, op1=ALU.add), reads=[b_o, b_dm, sm[a0][1]], writes=[b_o])
                    Mp, b_Mp = sm["Mp"]
                    wi, b_wi = sm["wi"]
                    fw.op("dve", lambda e: e.tensor_tensor(out=Mp[:], in0=sm["mnext"][0][:], in1=blast[:], op=ALU.subtract),
                          reads=[sm["mnext"][1], b_blast], writes=[b_Mp])
                    fw.op("dve", lambda e: e.tensor_tensor(out=wi[:], in0=sm["mbef"][0][:], in1=Mp[:], op=ALU.subtract),
                          reads=[sm["mbef"][1], b_Mp], writes=[b_wi])
                    fw.op("act", lambda e: e.activation(out=wi[:], in_=wi[:], func=AF.Exp), reads=[b_wi], writes=[b_wi])
                    mpb = Mp[:].unsqueeze(2).to_broadcast([8, NCH, 64])
                    fw.op("dve", lambda e: e.tensor_tensor(out=v3(IG), in0=v3(IG), in1=mpb, op=ALU.subtract), reads=[b_IG, b_Mp], writes=[b_IG])
                    fw.op("act", lambda e: e.activation(out=IG[:], in_=IG[:], func=AF.Exp), reads=[b_IG], writes=[b_IG])
                    fw.op("dve", lambda e: e.tensor_tensor(out=v3(NB), in0=v3(NB), in1=mpb, op=ALU.subtract), reads=[b_NB, b_Mp], writes=[b_NB])
                    fw.op("act", lambda e: e.activation(out=NB[:], in_=NB[:], func=AF.Exp), reads=[b_NB], writes=[b_NB])
                    for blk in range(3):
                        tk, b_tk = tk_ps[blk % 2]
                        c0 = blk * 32
                        nchb = min(32, NCH - c0)
                        for cc in range(nchb):
                            c = c0 + cc
                            fw.op("pe", lambda e: e.transpose(tk[:, cc, 0:8], IG[:, c * 64:(c + 1) * 64], self.ident_f[0:8, 0:8]),
                                  reads=[b_IG, self.b_ident], writes=[b_tk])
                            fw.op("pe", lambda e: e.transpose(tk[:, cc, 8:16], NB[:, c * 64:(c + 1) * 64], self.ident_f[0:8, 0:8]),
                                  reads=[b_NB, self.b_ident], writes=[b_tk])
                        fw.op("dve", lambda e: e.tensor_copy(out=tokT[:, c0:c0 + nchb, :], in_=tk[:, 0:nchb, :]), reads=[b_tk], writes=[b_tokT])
                    fw.op("dve", lambda e: e.tensor_tensor(out=rbd[:], in0=wi[:].unsqueeze(1).to_broadcast([8, 8, NCH]),
                                                           in1=self.ident_f[0:8, 0:8].unsqueeze(2).to_broadcast([8, 8, NCH]), op=ALU.mult),
                          reads=[b_wi, self.b_ident], writes=[b_rbd])
                    rflat = rbd[:].rearrange("p a c -> p (a c)")
                    wflat = WIbc[:].rearrange("p a c -> p (a c)")
                    for i, (n0, n1) in enumerate(((0, 512), (512, 8 * NCH))):
                        wp, b_wp = wi_ps[i]
                        fw.op("pe", lambda e: e.matmul(wp[:, 0:n1 - n0], lhsT=ones8[:], rhs=rflat[:, n0:n1], start=True, stop=True),
                              reads=[b_ones8, b_rbd], writes=[b_wp])
                        fw.op("dve", lambda e: e.tensor_copy(out=wflat[:, n0:n1], in_=wp[:, 0:n1 - n0]), reads=[b_wp], writes=[b_WIbc])
            with fw.scope() as sc:
                masks = []
                for d, sgn in ((0, -1), (1, 1)):
                    m_, b_m = sc.sb("cmask", [64, 64], F32)
                    fw.op("pool", lambda e: e.memset(m_[:], 1.0), writes=[b_m])
                    fw.op("pool", lambda e: e.affine_select(out=m_[:], in_=m_[:], pattern=[[-sgn, 64]], compare_op=ALU.is_ge, fill=0.0,
                                                             base=0, channel_multiplier=sgn), reads=[b_m], writes=[b_m])
                    masks.append((m_, b_m))
                wout, b_wout = sc.sb("wout", [128, 8, D], BF16)
                for hh in range(2):
                    fw.dma("pool", lambda e: e.dma_start(
                        out=wout[:, hh * 4:(hh + 1) * 4, :],
                        in_=self.mlstm_w_out[j, hh * 512:(hh + 1) * 512, :].rearrange("(k p) n -> p k n", p=128)),
                        writes=[b_wout], waw=False)
                WN, b_WN = self.load_row(sc, "wnrow", self.mlstm_norm[j:j + 1, :])
                g1 = {bi: self.load_row(sc, "g1row", self.mod_row(l, bi, "g1")), 2: self.load_row(sc, "g1rowc", self.mod_row(l, 2, "g1"))}
                qkcs = [sc.sb("qkc", [128, 8, 64], BF16) for _ in range(3)]
                vchs = [sc.sb("vch", [64, 4, 257], BF16) for _ in range(3)]
                for vch, b_vch in vchs:
                    fw.op("pool", lambda e: e.memset(vch[:, :, 256:257], 1.0), writes=[b_vch])
                kws = [sc.sb("kw", [64, 4, 128], BF16) for _ in range(2)]
                ptms = [sc.sb("ptm", [64, 4, 64], BF16) for _ in range(2)]
                ptf, b_ptf = sc.sb("ptf", [64, 4, 64], F32)
                Cst = [sc.sb("Cst", [128, 257], F32) for _ in range(4)]
                Dst = [sc.sb("Dst", [128, 257], BF16) for _ in range(4)]
                dds = [sc.sb("dd", [64, 2], F32) for _ in range(4)]
                hchs = [sc.sb("hch", [64, 4, 256], F32) for _ in range(2)]
                hfchs = [sc.sb("hfch", [64, 4, 256], F32) for _ in range(2)]
                sqt, b_sqt = sc.sb("sqt", [64, 4, 256], BF16)
                rss = [sc.sb("rs", [64, 12], F32) for _ in range(2)]
                aws = [sc.sb("aw", [64, D], F32) for _ in range(2)]
                pending_ro = [None]
                a16s = [sc.sb("a16", [64, D], BF16) for _ in range(2)]
                aTs = [sc.sb("aT", [128, 8, 128], BF16) for _ in range(2)]
                oTts = [sc.sb("oTt", [128, 8, 128], BF16) for _ in range(2)]
                xts = [sc.sb("xres", [128, D], F32) for _ in range(2)]
                tmps = [sc.sb("xtmp", [128, D], F32) for _ in range(2)]
                pt_ps, b_ptps = sc.ps("pt_ps", [128, 512])
                o_ps = [sc.ps("o_ps", [128, 512]) for _ in range(2)]
                u_ps = [sc.ps("u_ps", [128, 512]) for _ in range(2)]
                kw_ps, b_kwps = sc.ps("kw_ps", [64, 4, 128], BF16)
                aT_ps, b_aTps = sc.ps("aT_ps", [128, 8, 128], BF16)
                y_ps, b_yps = sc.ps("y_ps", [128, 512])
                cnt = dict(q=0, pt=0, o=0, u=0, h=0, t=0)
                for d in range(2):
                    cm, b_cm = masks[d]
                    for h in range(4):
                        fw.op("pool", lambda e: e.memset(Cst[h][0][:], 0.0), writes=[Cst[h][1]])
                        fw.op("pool", lambda e: e.memset(Dst[h][0][:], 0.0), writes=[Dst[h][1]])
                    ordr = order[d]

                    def load_chunk(si):
                        c = ordr[si]
                        qkc, b_qkc = qkcs[cnt["q"] % 3]
                        vch, b_vch = vchs[cnt["q"] % 3]
                        cnt["q"] += 1
                        fw.dma("sp", lambda e: e.dma_start(out=qkc[:], in_=qk_d[c]), reads=[b_qk], writes=[b_qkc])
                        fw.dma("sp", lambda e: e.dma_start(out=vch[:, :, 0:256], in_=v_d[c * 64:(c + 1) * 64, :].rearrange("p (h d) -> p h d", h=4)),
                               reads=[b_v], writes=[b_vch])
                        return (qkc, b_qkc, vch, b_vch)
                    nxt = load_chunk(0)
                    for si, c in enumerate(ordr):
                        qkc, b_qkc, vch, b_vch = nxt
                        if si + 1 < NCH:
                            nxt = load_chunk(si + 1)
                        kw, b_kw = kws[si % 2]
                        ptm, b_ptm = ptms[si % 2]
                        for h in range(4):
                            fw.op("pe", lambda e: e.transpose(kw_ps[:, h, :], qkc[:, 4 + h, :], self.ident_bf[:]),
                                  reads=[b_qkc, self.b_ident], writes=[b_kwps])
                        for h in range(4):
                            fw.op("pe", lambda e: e.matmul(pt_ps[0:64, h * 64:(h + 1) * 64], lhsT=qkc[:, 4 + h, :], rhs=qkc[:, h, :],
                                                           start=True, stop=True), reads=[b_qkc], writes=[b_ptps])
                        wk4 = tokT[:, c, d * 4:d * 4 + 4].unsqueeze(2)
                        fw.op("dve", lambda e: e.tensor_tensor(out=kw[:], in0=kw_ps[:], in1=wk4.to_broadcast([64, 4, 128]), op=ALU.mult),
                              reads=[b_kwps, b_tokT], writes=[b_kw])
                        ptv = pt_ps[0:64, 0:256].rearrange("p (h j) -> p h j", h=4)
                        fw.op("dve", lambda e: e.tensor_tensor(out=ptf[:], in0=ptv, in1=wk4.to_broadcast([64, 4, 64]), op=ALU.mult),
                              reads=[b_ptps, b_tokT], writes=[b_ptf])
                        fw.op("dve", lambda e: e.tensor_tensor(out=ptm[:], in0=ptf[:], in1=cm[:].unsqueeze(1).to_broadcast([64, 4, 64]), op=ALU.mult),
                              reads=[b_ptf, b_cm], writes=[b_ptm])
                        hch, b_hch = hchs[si % 2]
                        if d == 1:
                            hfch, b_hfch = hfchs[si % 2]
                            fw.dma("sp", lambda e: e.dma_start(out=hfch[:], in_=hf_d[c * 64:(c + 1) * 64, :].rearrange("p (h d) -> p h d", h=4)),
                                   reads=[b_hf], writes=[b_hfch])
                        for h in range(4):
                            r = d * 4 + h
                            up_, b_up = u_ps[cnt["u"] % 2]
                            cnt["u"] += 1
                            C_, b_C = Cst[h]
                            fw.op("pe", lambda e: e.matmul(up_[:, 0:257], lhsT=kw[:, h, :], rhs=vch[:, h, :], start=True, stop=True),
                                  reads=[b_kw, b_vch], writes=[b_up])
                            fw.op("dve", lambda e: e.scalar_tensor_tensor(out=C_[:], in0=C_[:], scalar=WIbc[:, r, c:c + 1], in1=up_[:, 0:257],
                                                                          op0=ALU.mult, op1=ALU.add), reads=[b_C, b_WIbc, b_up], writes=[b_C])
                        for h in range(4):
                            r = d * 4 + h
                            op_, b_op = o_ps[cnt["o"] % 2]
                            cnt["o"] += 1
                            D_, b_D = Dst[h]
                            C_, b_C = Cst[h]
                            dd, b_dd = dds[h]
                            fw.op("pe", lambda e: e.matmul(op_[0:64, 0:257], lhsT=ptm[:, h, :], rhs=vch[:, h, :], start=True, stop=False),
                                  reads=[b_ptm, b_vch], writes=[b_op])
                            fw.op("pe", lambda e: e.matmul(op_[0:64, 0:257], lhsT=qkc[:, h, :], rhs=D_[:], start=False, stop=True),
                                  reads=[b_qkc, b_D], writes=[b_op])
                            if si + 1 < NCH:
                                cn = ordr[si + 1]
                                fw.op("act", lambda e: e.activation(out=D_[:], in_=C_[:], func=AF.Copy, scale=WIbc[:, r, cn:cn + 1]),
                                      reads=[b_C, b_WIbc], writes=[b_D])
                            fw.op("dve", lambda e: e.tensor_tensor(out=dd[:, 0:1], in0=op_[0:64, 256:257], in1=tokT[:, c, 8 + r:9 + r], op=ALU.max),
                                  reads=[b_op, b_tokT], writes=[b_dd])
                            fw.op("dve", lambda e: e.scalar_tensor_tensor(out=dd[:, 0:1], in0=op_[0:64, 256:257], scalar=-1.0, in1=dd[:, 0:1],
                                                                          op0=ALU.mult, op1=ALU.max), reads=[b_op, b_dd], writes=[b_dd])
                            fw.op("dve", lambda e: e.reciprocal(out=dd[:, 1:2], in_=dd[:, 0:1]), reads=[b_dd], writes=[b_dd])
                            if d == 0:
                                fw.op("act", lambda e: e.activation(out=hch[:, h, :], in_=op_[0:64, 0:256], func=AF.Copy, scale=dd[:, 1:2]),
                                      reads=[b_op, b_dd], writes=[b_hch])
                            else:
                                fw.op("dve", lambda e: e.scalar_tensor_tensor(out=hch[:, h, :], in0=op_[0:64, 0:256], scalar=dd[:, 1:2], in1=hfch[:, h, :],
                                                                              op0=ALU.mult, op1=ALU.add), reads=[b_op, b_dd, b_hfch], writes=[b_hch])
                        if d == 0:
                            fw.dma("sp", lambda e: e.dma_start(out=hf_d[c * 64:(c + 1) * 64, :].rearrange("p (h d) -> p h d", h=4), in_=hch[:]),
                                   reads=[b_hch], writes=[b_hf], waw=False)
                            continue
                        aw, b_aw = aws[si % 2]
                        rs, b_rs = rss[si % 2]
                        fw.op("pool", lambda e: e.tensor_tensor(out=aw[:], in0=hch[:].rearrange("p h d -> p (h d)"), in1=WN[0:64, :], op=ALU.mult),
                              reads=[b_hch, b_WN], writes=[b_aw])
                        for h in range(4):
                            fw.op("act", lambda e: e.activation(out=sqt[:, h, :], in_=hch[:, h, :], func=AF.Square, accum_out=rs[:, h:h + 1]),
                                  reads=[b_hch], writes=[b_sqt, b_rs])
                        fw.op("act", lambda e: e.activation(out=rs[:, 4:8], in_=rs[:, 0:4], func=AF.Ln, scale=1.0 / 256, bias=EPS),
                              reads=[b_rs], writes=[b_rs])
                        fw.op("act", lambda e: e.activation(out=rs[:, 8:12], in_=rs[:, 4:8], func=AF.Exp, scale=-0.5),
                              reads=[b_rs], writes=[b_rs])
                        if pending_ro[0] is not None:
                            pending_ro[0]()

                        def stage2(c=c, si=si, aw=aw, b_aw=b_aw, rs=rs, b_rs=b_rs):
                            t = c // 2
                            off = (c % 2) * 64
                            a16, b_a16 = a16s[si % 2]
                            fw.op("dve", lambda e: e.tensor_tensor(out=a16[:].rearrange("p (h d) -> p h d", h=4),
                                                                   in0=aw[:].rearrange("p (h d) -> p h d", h=4),
                                                                   in1=rs[:, 8:12].unsqueeze(2).to_broadcast([64, 4, 256]), op=ALU.mult),
                                  reads=[b_aw, b_rs], writes=[b_a16])
                            for kk in range(8):
                                fw.op("pe", lambda e: e.transpose(aT_ps[:, kk, off:off + 64], a16[:, kk * 128:(kk + 1) * 128], self.ident_bf[0:64, 0:64]),
                                      reads=[b_a16, self.b_ident], writes=[b_aTps])
                            if c % 2 == 1:
                                return
                            aT, b_aT = aTs[cnt["t"] % 2]
                            oTt, b_oTt = oTts[cnt["t"] % 2]
                            xt, b_xt = xts[cnt["t"] % 2]
                            tmp, b_tmp = tmps[cnt["t"] % 2]
                            cnt["t"] += 1
                            fw.dma("sp", lambda e: e.dma_start(out=oTt[:], in_=oT_d[t]), reads=[b_oT], writes=[b_oTt])
                            rap, b_rap = self.res_tile(bi, t)
                            fw.dma("sp", lambda e: e.dma_start(out=xt[:], in_=rap), reads=[b_rap], writes=[b_xt])
                            fw.op("dve", lambda e: e.tensor_tensor(out=aT[:], in0=aT_ps[:], in1=oTt[:], op=ALU.mult), reads=[b_aTps, b_oTt], writes=[b_aT])
                            grow, b_grow = g1[2 if t < 2 else bi]
                            for half in range(2):
                                for kk in range(8):
                                    fw.op("pe", lambda e: e.matmul(y_ps[:], lhsT=aT[:, kk, :], rhs=wout[:, kk, half * 512:(half + 1) * 512],
                                                                   start=(kk == 0), stop=(kk == 7)), reads=[b_aT, b_wout], writes=[b_yps])
                                hs = slice(half * 512, (half + 1) * 512)
                                fw.op("dve", lambda e: e.tensor_tensor(out=tmp[:, hs], in0=y_ps[:], in1=grow[:, hs], op=ALU.mult),
                                      reads=[b_yps, b_grow], writes=[b_tmp])
                                fw.op("pool", lambda e: e.tensor_tensor(out=tmp[:, hs], in0=tmp[:, hs], in1=xt[:, hs], op=ALU.add),
                                      reads=[b_tmp, b_xt], writes=[b_tmp])
                            fw.dma("sp", lambda e: e.dma_start(out=rap, in_=tmp[:]), reads=[b_tmp], writes=[b_rap], waw=False)
                        pending_ro[0] = stage2
                    if pending_ro[0] is not None:
                        pending_ro[0]()
                        pending_ro[0] = None

    def phase_final(self):
        fw = self.fw
        with fw.scope() as sc:
            W = self.norm_work(sc)
            nf, b_nf = self.load_row(sc, "nf", self.norm_final[0:1, :])
            zr, b_zr = sc.sb("zrow", [128, D], F32)
            fw.op("pool", lambda e: e.memset(zr[:], 0.0), writes=[b_zr])
            outs = [sc.sb("fo", [128, D], F32) for _ in range(2)]
            tiles = [(bi, t) for bi in range(self.nb) for t in range(NL // 128)]
            nxt_x = self.norm_load(W, self.outs[tiles[0][0]][0:128, :], self.b_outs[tiles[0][0]])
            for i, (bi, t) in enumerate(tiles):
                cur_x = nxt_x
                if i + 1 < len(tiles):
                    b2, t2 = tiles[i + 1]
                    nxt_x = self.norm_load(W, self.outs[b2][t2 * 128:(t2 + 1) * 128, :], self.b_outs[b2])
                o, b_o = outs[i % 2]
                self.norm_tile(W, cur_x, nf, b_nf, zr, b_zr, o[:], b_o)
                fw.dma("sp", lambda e: e.dma_start(out=self.outs[bi][t * 128:(t + 1) * 128, :], in_=o[:]),
                       reads=[b_o], writes=[self.b_outs[bi]], waw=False)


def rope_tables():
    grid_w = 64
    pairs = 16
    t = np.arange(NL)
    row = (t // grid_w).astype(np.float32)
    col = (t % grid_w).astype(np.float32)
    inv = (np.float32(10000.0) ** (-np.arange(pairs, dtype=np.float32) / np.float32(pairs))).astype(np.float32)
    ang = np.concatenate([row[:, None] * inv, col[:, None] * inv], axis=-1).astype(np.float32)
    cos = np.cos(ang).astype(np.float32).T
    sin = np.sin(ang).astype(np.float32).T
    cos64 = np.concatenate([cos, cos], axis=0)
    sin64 = np.concatenate([sin, sin], axis=0)
    return (np.ascontiguousarray(np.concatenate([cos64, cos64], axis=0)),
            np.ascontiguousarray(np.concatenate([sin64, sin64], axis=0)))


FULL_PLAN = [("mod",)] + [s for l in range(DEPTH) for s in (("mix", l), ("moe", l))] + [("final",)]


def make_in_maps(inputs, n_cores, nb):
    cos, sin = rope_tables()
    shared = {k: np.ascontiguousarray(v) for k, v in inputs.items() if k not in ("x", "c", "ctx", "c_ctx", "norm_final")}
    shared["c_ctx"] = np.ascontiguousarray(inputs["c_ctx"]).reshape(1, D)
    shared["norm_final"] = np.ascontiguousarray(inputs["norm_final"]).reshape(1, D)
    shared["rope_cos"] = cos
    shared["rope_sin"] = sin
    maps = []
    for c in range(n_cores):
        m = dict(shared)
        m["x"] = np.ascontiguousarray(inputs["x"][c * nb:(c + 1) * nb])
        m["c"] = np.ascontiguousarray(inputs["c"][c * nb:(c + 1) * nb])
        m["ctx"] = np.ascontiguousarray(inputs["ctx"][c * nb:(c + 1) * nb])
        maps.append(m)
    return maps


def kernel(**inputs):
    n_cores = 8
    nb = 2
    prog = Prog(nb, FULL_PLAN)
    maps = make_in_maps(inputs, n_cores, nb)
    res = run_bass_kernel_spmd(prog.nc, maps, core_ids=list(range(n_cores)))
    return np.stack([r[f"out{b}"] for r in res.results for b in range(nb)], axis=0).astype(np.float32)
```

```python
import contextlib
import numpy as np
import concourse.bass as bass
import concourse.mybir as mybir
from concourse.bass_utils import run_bass_kernel_spmd

F32 = mybir.dt.float32
BF16 = mybir.dt.bfloat16
U32 = mybir.dt.uint32
I32 = mybir.dt.int32
AF = mybir.ActivationFunctionType
ALU = mybir.AluOpType
AX = mybir.AxisListType

D = 1024
NL = 4096
NCX = 256
NT = NL + NCX
DEPTH = 4
EPS = 1e-6
NE = 16
ND = 6


class Buf:
    __slots__ = ("w", "r", "excl")

    def __init__(self, excl=False):
        self.w = {}
        self.r = {}
        self.excl = excl


class Scope:
    def __init__(self, fw):
        self.fw = fw
        self.es = contextlib.ExitStack()

    def __enter__(self):
        self.es.__enter__()
        return self

    def __exit__(self, *a):
        self.fw.barrier()
        return self.es.__exit__(*a)

    def sb(self, name, shape, dt):
        self.fw.uid += 1
        t = self.es.enter_context(self.fw.nc.sbuf_tensor(f"{name}_{self.fw.uid}", list(shape), dt))
        return t, Buf()

    def ps(self, name, shape, dt=F32):
        self.fw.uid += 1
        esz = 4 if dt == F32 else 2
        bank = 2048 // esz
        n = 1
        for d_ in shape[1:]:
            n *= d_
        nalloc = ((n + bank - 1) // bank) * bank
        t = self.es.enter_context(self.fw.nc.psum_tensor(f"{name}_{self.fw.uid}", [128, nalloc], dt))
        v = t[0:shape[0], 0:n]
        if len(shape) == 3:
            v = v.rearrange("p (a b) -> p a b", a=shape[1], b=shape[2])
        elif len(shape) != 2:
            raise ValueError(shape)
        return v, Buf(excl=True)


class FW:
    def __init__(self, nc, es):
        self.nc = nc
        self.es = es
        self.uid = 0
        self.e = dict(pe=nc.tensor, act=nc.scalar, pool=nc.gpsimd, dve=nc.vector, sp=nc.sync)
        self.semh = {}
        self.cnt = {}
        self.seen = {k: {} for k in self.e}
        for k in self.e:
            key = "c_" + k
            self.semh[key] = es.enter_context(nc.semaphore(key))
            self.cnt[key] = 0
        self.drr = {}
        for q in ("sp", "pool", "act", "poolw"):
            self.drr[q] = 0
            for i in range(ND):
                key = f"d_{q}{i}"
                self.semh[key] = es.enter_context(nc.semaphore(key))
                self.cnt[key] = 0
        self.n_ins = 0
        self.n_wait = 0

    def scope(self):
        return Scope(self)

    def _wait(self, eng, deps):
        seen = self.seen[eng]
        for k, v in deps.items():
            if seen.get(k, 0) >= v:
                continue
            self.e[eng].wait_ge(self.semh[k], v)
            seen[k] = v
            self.n_wait += 1

    def op(self, eng, fn, reads=(), writes=()):
        deps = {}
        own = "c_" + eng
        for b in reads:
            for k, v in b.w.items():
                if deps.get(k, 0) < v:
                    deps[k] = v
            if b.excl:
                for k, v in b.r.items():
                    if k != own and deps.get(k, 0) < v:
                        deps[k] = v
        for b in writes:
            for src in (b.w, b.r):
                for k, v in src.items():
                    if k == own:
                        continue
                    if deps.get(k, 0) < v:
                        deps[k] = v
        self._wait(eng, deps)
        ins = fn(self.e[eng])
        self.cnt[own] += 1
        ins.then_inc(self.semh[own], 1)
        v = self.cnt[own]
        for b in reads:
            b.r[own] = v
        for b in writes:
            b.w[own] = v
        self.n_ins += 1
        return ins

    def dma(self, q, fn, reads=(), writes=(), waw=True):
        deps = {}
        for b in reads:
            for k, v in b.w.items():
                if deps.get(k, 0) < v:
                    deps[k] = v
        for b in writes:
            for src in ((b.w, b.r) if waw else (b.r,)):
                for k, v in src.items():
                    if deps.get(k, 0) < v:
                        deps[k] = v
        i = self.drr[q]
        self.drr[q] = (i + 1) % ND
        key = f"d_{q}{i}"
        if self.cnt[key] > 0 and deps.get(key, 0) < self.cnt[key]:
            deps[key] = self.cnt[key]
        q = "pool" if q == "poolw" else q
        self._wait(q, deps)
        ins = fn(self.e[q])
        self.cnt[key] += 16
        ins.then_inc(self.semh[key], 16)
        v = self.cnt[key]
        for b in reads:
            b.r[key] = v
        for b in writes:
            b.w[key] = v
        self.n_ins += 1
        return ins

    def barrier(self):
        allv = {k: v for k, v in self.cnt.items() if v > 0}
        for eng in self.e:
            self._wait(eng, allv)

    def finish(self):
        self._wait("sp", {k: v for k, v in self.cnt.items() if v > 0})


MOD_OFF = dict(sh1=0, sc1=1, g1=2, sh2=3, sc2=4, g2=5)


class Prog:
    def __init__(self, nb, plan, dbg=False):
        self.nb = nb
        self.plan = plan
        nc = self.nc = bass.Bass("TRN2", target_bir_lowering=False)
        dt = nc.dram_tensor

        def ein(name, shape, d=F32):
            return dt(name, list(shape), d, kind="ExternalInput").ap()

        self.x = ein("x", [nb, NL, D])
        self.c = ein("c", [nb, D])
        self.ctx = ein("ctx", [nb, NCX, D])
        self.c_ctx = ein("c_ctx", [1, D])
        self.w_mod = ein("w_mod", [DEPTH, D, 6 * D])
        self.b_mod = ein("b_mod", [DEPTH, 6 * D])
        self.norm_mix = ein("norm_mix", [DEPTH, D])
        self.norm_ffn = ein("norm_ffn", [DEPTH, D])
        self.mlstm_w_in = ein("mlstm_w_in", [2, D, 3088])
        self.mlstm_b_gate = ein("mlstm_b_gate", [2, 16])
        self.mlstm_norm = ein("mlstm_norm", [2, D])
        self.mlstm_w_out = ein("mlstm_w_out", [2, D, D])
        self.attn_w_in = ein("attn_w_in", [2, D, 1536])
        self.attn_q_norm = ein("attn_q_norm", [2, 64])
        self.attn_k_norm = ein("attn_k_norm", [2, 64])
        self.attn_w_out = ein("attn_w_out", [2, D, D])
        self.moe_router = ein("moe_router", [DEPTH, D, NE])
        self.moe_w_gate = ein("moe_w_gate", [DEPTH, NE, D, D])
        self.moe_w_up = ein("moe_w_up", [DEPTH, NE, D, D])
        self.moe_w_down = ein("moe_w_down", [DEPTH, NE, D, D])
        self.norm_final = ein("norm_final", [1, D])
        self.rope_cos = ein("rope_cos", [128, NL])
        self.rope_sin = ein("rope_sin", [128, NL])
        self.outs = [dt(f"out{b}", [NL, D], F32, kind="ExternalOutput").ap() for b in range(nb)]
        self.b_outs = [Buf() for _ in range(nb)]
        self.xcs = [dt(f"xc_res{b}", [NCX, D], F32).ap() for b in range(nb)]
        self.b_xcs = [Buf() for _ in range(nb)]
        self.mod_d = dt("mod_d", [DEPTH, 3, 6 * D], F32).ap()
        self.b_mod_d = Buf()
        self.h2l = [dt(f"h2l{b}", [NL, D], BF16).ap() for b in range(nb)]
        self.h2c = [dt(f"h2c{b}", [NCX, D], BF16).ap() for b in range(nb)]
        self.b_h2 = Buf()
        self.tkv = dt("tkv", [2 * nb, NE, 512], F32).ap()
        self.tki = dt("tki", [2 * nb, NE, 512], U32).ap()
        self.b_tk = Buf()
        self.dbg = dbg
        if dbg:
            self.xc_out = dt("xc_out", [nb, NCX, D], F32, kind="ExternalOutput").ap()
            self.mod_out = dt("mod_out", [DEPTH, 3, 6 * D], F32, kind="ExternalOutput").ap()

        with contextlib.ExitStack() as es:
            fw = self.fw = FW(nc, es)
            self.consts(es)
            self.phase_init()
            for step in plan:
                if step[0] == "mod":
                    self.phase_mod()
                elif step[0] == "moe":
                    self.phase_moe(step[1])
                elif step[0] == "mix":
                    for bi in range(nb):
                        if step[1] % 2 == 1:
                            self.phase_attn(step[1], bi)
                        else:
                            self.phase_mlstm(step[1], bi)
                elif step[0] == "final":
                    self.phase_final()
            if dbg:
                for b in range(nb):
                    fw.dma("sp", lambda e: e.dma_start(out=self.xc_out[b, :, :], in_=self.xcs[b][:, :]),
                           reads=[self.b_xcs[b]])
                fw.dma("sp", lambda e: e.dma_start(out=self.mod_out[:, :, :], in_=self.mod_d[:, :, :]),
                       reads=[self.b_mod_d])
            fw.finish()
            self.stats = (fw.n_ins, fw.n_wait)

    def res_tile(self, bi, t):
        if t < 2:
            return self.xcs[bi][t * 128:(t + 1) * 128, :], self.b_xcs[bi]
        return self.outs[bi][(t - 2) * 128:(t - 1) * 128, :], self.b_outs[bi]

    def consts(self, es):
        fw = self.fw
        nc = self.nc
        self.ident_bf = es.enter_context(nc.sbuf_tensor("ident_bf", [128, 128], BF16))
        self.ident_f = es.enter_context(nc.sbuf_tensor("ident_f", [128, 128], F32))
        self.b_ident = Buf()
        for t in (self.ident_bf, self.ident_f):
            fw.op("pool", lambda e: e.memset(t[:], 1.0), writes=[self.b_ident])
            fw.op("pool", lambda e: e.affine_select(out=t[:], in_=t[:], pattern=[[-1, 128]],
                                                     compare_op=ALU.is_equal, fill=0.0, base=0,
                                                     channel_multiplier=1),
                  reads=[self.b_ident], writes=[self.b_ident])

    def phase_init(self):
        fw = self.fw
        for bi in range(self.nb):
            for h in range(4):
                fw.dma("sp", lambda e: e.dma_start(out=self.outs[bi][h * 1024:(h + 1) * 1024, :],
                                                   in_=self.x[bi, h * 1024:(h + 1) * 1024, :]),
                       writes=[self.b_outs[bi]], waw=False)
            fw.dma("sp", lambda e: e.dma_start(out=self.xcs[bi][:, :], in_=self.ctx[bi, :, :]), writes=[self.b_xcs[bi]])

    def phase_mod(self):
        fw = self.fw
        nb = self.nb
        with fw.scope() as sc:
            cT, b_cT = sc.sb("cT", [128, 8, 3], F32)
            sT, b_sT = sc.sb("sT", [128, 8, 3], F32)
            pss = [sc.ps("modps", [128, 512]) for _ in range(2)]
            wts = [sc.sb("wmod", [128, 8, 512], F32) for _ in range(3)]
            bts = [sc.sb("bmod", [3, 512], F32) for _ in range(3)]
            mrs = [sc.sb("mrow", [3, 512], F32) for _ in range(2)]
            rows = [self.c[min(r, nb - 1), :] for r in range(2)] + [self.c_ctx[0, :]]
            for r in range(3):
                fw.dma("sp", lambda e: e.dma_start(out=cT[:, :, r], in_=rows[r].rearrange("(k p) -> p k", p=128),
                                                   allow_slow_non_contiguous=True), writes=[b_cT])
            fw.op("act", lambda e: e.activation(out=sT[:], in_=cT[:], func=AF.Silu), reads=[b_cT], writes=[b_sT])
            chunks = [(l, n) for l in range(DEPTH) for n in range(12)]

            def issue_load(i):
                l, n = chunks[i]
                n0 = n * 512
                (wt, b_wt), (bt, b_bt) = wts[i % 3], bts[i % 3]
                fw.dma("sp", lambda e: e.dma_start(
                    out=wt[:], in_=self.w_mod[l, :, n0:n0 + 512].rearrange("(k p) n -> p k n", p=128)),
                    writes=[b_wt])
                fw.dma("sp", lambda e: e.dma_start(
                    out=bt[:], in_=self.b_mod[l:l + 1, n0:n0 + 512].partition_broadcast(3)), writes=[b_bt])
            issue_load(0)
            issue_load(1)
            for i, (l, n) in enumerate(chunks):
                if i + 2 < len(chunks):
                    issue_load(i + 2)
                n0 = n * 512
                (wt, b_wt), (bt, b_bt), (mr, b_mr) = wts[i % 3], bts[i % 3], mrs[i % 2]
                ps, b_ps = pss[i % 2]
                for k in range(8):
                    fw.op("pe", lambda e: e.matmul(ps[0:3, :], lhsT=sT[:, k, :], rhs=wt[:, k, :],
                                                   start=(k == 0), stop=(k == 7)),
                          reads=[b_sT, b_wt], writes=[b_ps])
                fw.op("dve", lambda e: e.tensor_tensor(out=mr[:], in0=ps[0:3, :], in1=bt[:], op=ALU.add),
                      reads=[b_ps, b_bt], writes=[b_mr])
                fw.dma("sp", lambda e: e.dma_start(out=self.mod_d[l, :, n0:n0 + 512], in_=mr[:]),
                       reads=[b_mr], writes=[self.b_mod_d], waw=False)

    def load_row(self, sc, name, src_row):
        t, b = sc.sb(name, [128, D], F32)
        self.fw.dma("sp", lambda e: e.dma_start(out=t[:], in_=src_row.partition_broadcast(128)),
                    reads=[self.b_mod_d], writes=[b])
        return t, b

    def mod_row(self, l, r, which):
        o = MOD_OFF[which] * D
        return self.mod_d[l, r:r + 1, o:o + D]

    def make_arow(self, sc, l, r, which_sc, norm_w_row):
        fw = self.fw
        a, b_a = self.load_row(sc, "arow", self.mod_row(l, r, which_sc))
        nw, b_nw = self.load_row(sc, "nwrow", norm_w_row)
        fw.op("dve", lambda e: e.scalar_tensor_tensor(out=a[:], in0=a[:], scalar=1.0, in1=nw[:],
                                                      op0=ALU.add, op1=ALU.mult),
              reads=[b_a, b_nw], writes=[b_a])
        return a, b_a

    def norm_load(self, W, src_ap, b_src):
        xt, b_xt = W["xt"][W["i"] % 3]
        W["i"] += 1
        self.fw.dma("sp", lambda e: e.dma_start(out=xt[:], in_=src_ap), reads=[b_src], writes=[b_xt])
        return xt, b_xt

    def norm_tile(self, W, xtb, arow, b_arow, srow, b_srow, h_out, b_h):
        fw = self.fw
        xt, b_xt = xtb
        junk, b_junk = W["junk"]
        st, b_st = W["st"]
        tmp, b_tmp = W["tmp"]
        fw.op("act", lambda e: e.activation(out=junk[:], in_=xt[:], func=AF.Square, accum_out=st[:, 0:1]),
              reads=[b_xt], writes=[b_junk, b_st])
        fw.op("dve", lambda e: e.tensor_scalar(out=st[:, 1:2], in0=st[:, 0:1], scalar1=1.0 / D, scalar2=EPS,
                                               op0=ALU.mult, op1=ALU.add), reads=[b_st], writes=[b_st])
        fw.op("act", lambda e: e.activation(out=st[:, 2:3], in_=st[:, 1:2], func=AF.Sqrt), reads=[b_st], writes=[b_st])
        fw.op("dve", lambda e: e.reciprocal(out=st[:, 3:4], in_=st[:, 2:3]), reads=[b_st], writes=[b_st])
        fw.op("dve", lambda e: e.scalar_tensor_tensor(out=tmp[:], in0=xt[:], scalar=st[:, 3:4], in1=arow[:],
                                                      op0=ALU.mult, op1=ALU.mult),
              reads=[b_xt, b_st, b_arow], writes=[b_tmp])
        fw.op("pool", lambda e: e.tensor_tensor(out=h_out, in0=tmp[:], in1=srow[:], op=ALU.add),
              reads=[b_tmp, b_srow], writes=[b_h])
        return xt, b_xt

    def norm_work(self, sc):
        return dict(i=0, xt=[sc.sb("xt", [128, D], F32) for _ in range(3)], junk=sc.sb("junk", [128, D], BF16),
                    st=sc.sb("st", [128, 4], F32), tmp=sc.sb("tmp", [128, D], F32))

    def phase_moe(self, l):
        fw = self.fw
        nb = self.nb
        last = l == DEPTH - 1
        sets = [(bi, 0) for bi in range(nb)] + ([] if last else [(bi, 1) for bi in range(nb)])
        tsc = fw.scope()
        tsc.__enter__()
        tk_bufs = {}
        for si, (bi, is_ctx) in enumerate(sets):
            ntok = NCX if is_ctx else NL
            cap = 2 * ntok // NE
            tk_bufs[si] = ([tsc.sb("affT", [NE, ntok], F32) for _ in range(2)], tsc.sb("vals", [NE, cap], F32),
                           tsc.sb("idxs", [NE, cap], U32), cap)
        for si, (bi, is_ctx) in enumerate(sets):
            ntok = NCX if is_ctx else NL
            cap = 2 * ntok // NE
            r = 2 if is_ctx else bi
            affT = tk_bufs[si][0]
            with fw.scope() as sc:
                W = self.norm_work(sc)
                arow, b_arow = self.make_arow(sc, l, r, "sc2", self.norm_ffn[l:l + 1, :])
                srow, b_srow = self.load_row(sc, "srow", self.mod_row(l, r, "sh2"))
                wr, b_wr = sc.sb("wr", [128, 8, NE], F32)
                fw.dma("sp", lambda e: e.dma_start(
                    out=wr[:], in_=self.moe_router[l, :, :].rearrange("(k p) n -> p k n", p=128)), writes=[b_wr])
                h32s = [sc.sb("h32", [128, D], F32) for _ in range(2)]
                h16s = [sc.sb("h16", [128, D], BF16) for _ in range(2)]
                hT, b_hT = sc.sb("hT32", [128, 8, 128], F32)
                tps = [sc.ps("tps", [128, 4, 128], F32) for _ in range(2)]
                lps, b_lps = sc.ps("lps", [128, NE], F32)
                aps, b_aps = sc.ps("aps", [NE, 128], F32)
                sm, b_sm = sc.sb("sm", [128, 4], F32)
                ex, b_ex = sc.sb("ex", [128, NE], F32)
                aff, b_aff = sc.sb("aff", [128, NE], F32)

                def src_of(t):
                    return (self.xcs[bi][t * 128:(t + 1) * 128, :], self.b_xcs[bi]) if is_ctx else \
                           (self.outs[bi][t * 128:(t + 1) * 128, :], self.b_outs[bi])
                ntile = ntok // 128
                nxt_x = self.norm_load(W, *src_of(0))
                for t in range(ntile):
                    cur_x = nxt_x
                    if t + 1 < ntile:
                        nxt_x = self.norm_load(W, *src_of(t + 1))
                    h32, b_h32 = h32s[t % 2]
                    h16, b_h16 = h16s[t % 2]
                    self.norm_tile(W, cur_x, arow, b_arow, srow, b_srow, h32[:], b_h32)
                    fw.op("act", lambda e: e.activation(out=h16[:], in_=h32[:], func=AF.Copy), reads=[b_h32], writes=[b_h16])
                    dst = self.h2c[bi][t * 128:(t + 1) * 128, :] if is_ctx else self.h2l[bi][t * 128:(t + 1) * 128, :]
                    fw.dma("sp", lambda e: e.dma_start(out=dst, in_=h16[:]), reads=[b_h16], writes=[self.b_h2], waw=False)
                    for half in range(2):
                        tp, b_tp = tps[half]
                        for k in range(4):
                            kk = half * 4 + k
                            fw.op("pe", lambda e: e.transpose(tp[:, k, :], h32[:, kk * 128:(kk + 1) * 128], self.ident_f[:]),
                                  reads=[b_h32, self.b_ident], writes=[b_tp])
                        fw.op("dve", lambda e: e.tensor_copy(out=hT[:, half * 4:(half + 1) * 4, :], in_=tp[:]),
                              reads=[b_tp], writes=[b_hT])
                    for k in range(8):
                        fw.op("pe", lambda e: e.matmul(lps[:], lhsT=hT[:, k, :], rhs=wr[:, k, :], start=(k == 0), stop=(k == 7)),
                              reads=[b_hT, b_wr], writes=[b_lps])
                    fw.op("dve", lambda e: e.tensor_reduce(out=sm[:, 0:1], in_=lps[:], axis=AX.X, op=ALU.max, negate=True),
                          reads=[b_lps], writes=[b_sm])
                    fw.op("act", lambda e: e.activation(out=ex[:], in_=lps[:], func=AF.Exp, bias=sm[:, 0:1], accum_out=sm[:, 1:2]),
                          reads=[b_lps, b_sm], writes=[b_ex, b_sm])
                    fw.op("dve", lambda e: e.reciprocal(out=sm[:, 2:3], in_=sm[:, 1:2]), reads=[b_sm], writes=[b_sm])
                    fw.op("dve", lambda e: e.tensor_scalar(out=aff[:], in0=ex[:], scalar1=sm[:, 2:3], scalar2=None, op0=ALU.mult),
                          reads=[b_ex, b_sm], writes=[b_aff])
                    fw.op("pe", lambda e: e.transpose(aps[:], aff[:], self.ident_f[:]), reads=[b_aff, self.b_ident], writes=[b_aps])
                    fw.op("dve", lambda e: e.tensor_copy(out=affT[0][0][:, t * 128:(t + 1) * 128], in_=aps[:]),
                          reads=[b_aps], writes=[affT[0][1]])
        max_rounds = max(v[3] // 8 for v in tk_bufs.values())
        for rd in range(max_rounds):
            for si in range(len(sets)):
                affT, (vals, b_vals), (idxs, b_idxs), cap = tk_bufs[si]
                if rd >= cap // 8:
                    continue
                cur, b_cur = affT[rd % 2]
                nxt, b_nxt = affT[(rd + 1) % 2]
                v8 = vals[:, rd * 8:(rd + 1) * 8]
                fw.op("dve", lambda e: e.max(out=v8, in_=cur[:]), reads=[b_cur], writes=[b_vals])
                fw.op("dve", lambda e: e.max_index(out=idxs[:, rd * 8:(rd + 1) * 8], in_max=v8, in_values=cur[:]),
                      reads=[b_cur, b_vals], writes=[b_idxs])
                if rd < cap // 8 - 1:
                    fw.op("dve", lambda e: e.match_replace(out=nxt[:], in_to_replace=v8, in_values=cur[:], imm_value=-1.0),
                          reads=[b_cur, b_vals], writes=[b_nxt])
        for si in range(len(sets)):
            affT, (vals, b_vals), (idxs, b_idxs), cap = tk_bufs[si]
            fw.dma("sp", lambda e: e.dma_start(out=self.tkv[si, :, 0:cap], in_=vals[:]), reads=[b_vals], writes=[self.b_tk], waw=False)
            fw.dma("sp", lambda e: e.dma_start(out=self.tki[si, :, 0:cap], in_=idxs[:]), reads=[b_idxs], writes=[self.b_tk], waw=False)
        tsc.__exit__(None, None, None)
        with fw.scope() as sc:
            wbufs = [[sc.sb(f"w{m}", [128, 8, D], BF16) for m in range(3)] for _ in range(2)]
            grows = {}
            for bi in range(nb):
                grows[(bi, 0)] = self.load_row(sc, "g2row", self.mod_row(l, bi, "g2"))
            if not last:
                g = self.load_row(sc, "g2rowc", self.mod_row(l, 2, "g2"))
                for bi in range(nb):
                    grows[(bi, 1)] = g
            sl = {}
            for si, (bi, is_ctx) in enumerate(sets):
                ntok = NCX if is_ctx else NL
                cap = 2 * ntok // NE
                nblk = max(1, cap // 128)
                rows = min(cap, 128)
                ix, b_ix = sc.sb("ix", [128, NE, nblk], U32)
                gv, b_gv = sc.sb("gv", [128, NE, nblk], F32)
                fw.dma("sp", lambda e: e.dma_start(out=ix[0:rows], in_=self.tki[si, :, 0:cap].rearrange("e (p k) -> p e k", k=nblk),
                                                   allow_slow_non_contiguous=True), reads=[self.b_tk], writes=[b_ix])
                fw.dma("sp", lambda e: e.dma_start(out=gv[0:rows], in_=self.tkv[si, :, 0:cap].rearrange("e (p k) -> p e k", k=nblk),
                                                   allow_slow_non_contiguous=True), reads=[self.b_tk], writes=[b_gv])
                sl[si] = (ix, b_ix, gv, b_gv, nblk, rows)
            xgs = [sc.sb("xg", [128, D], BF16) for _ in range(12)]
            xgT = [sc.sb("xgT", [128, 8, 512], BF16) for _ in range(3)]
            actT = [sc.sb("actT", [128, 8, 512], BF16) for _ in range(2)]
            sas = [sc.sb("sa", [128, 512], F32) for _ in range(2)]
            yscs = [sc.sb("ysc", [128, D], F32) for _ in range(3)]
            tpp = [sc.ps("tpp", [128, 8, 128], BF16) for _ in range(2)]
            a_ps = [sc.ps("a_ps", [128, 512]) for _ in range(2)]
            u_ps = [sc.ps("u_ps", [128, 512]) for _ in range(2)]
            y_ps = [sc.ps("y_ps", [128, 512]) for _ in range(2)]
            cnt = dict(xg=0, tp=0, au=0, y=0, ysc=0)
            wsrc = (self.moe_w_gate, self.moe_w_up, self.moe_w_down)

            def load_w(ex_i):
                wb = wbufs[ex_i % 2]
                for m in range(3):
                    for hh in range(2):
                        fw.dma("poolw", lambda e: e.dma_start(
                            out=wb[m][0][:, hh * 4:(hh + 1) * 4, :],
                            in_=wsrc[m][l, ex_i, hh * 512:(hh + 1) * 512, :].rearrange("(k p) n -> p k n", p=128)),
                            writes=[wb[m][1]], waw=False)

            items = [(ex_i, si) for ex_i in range(NE) for si in range(len(sets))]

            xg_of = {}

            def stage_gather(j):
                ex_i, si = items[j]
                bi, is_ctx = sets[si]
                ix, b_ix, gv, b_gv, nblk, rows = sl[si]
                src = self.h2c[bi][:, :] if is_ctx else self.h2l[bi][:, :]
                xg_of[j] = []
                for k in range(nblk):
                    xg, b_xg = xgs[cnt["xg"] % len(xgs)]
                    cnt["xg"] += 1
                    xg_of[j].append((xg, b_xg))
                    fw.dma("pool", lambda e: e.indirect_dma_start(
                        out=xg[0:rows, :], out_offset=None, in_=src,
                        in_offset=bass.IndirectOffsetOnAxis(ap=ix[0:rows, ex_i, k:k + 1], axis=0)),
                        reads=[b_ix, self.b_h2], writes=[b_xg])

            def stage_tr(j):
                ex_i, si = items[j]
                ix, b_ix, gv, b_gv, nblk, rows = sl[si]
                xT, b_xT = xgT[j % 3]
                for k in range(nblk):
                    xg, b_xg = xg_of[j][k]
                    tp, b_tp = tpp[cnt["tp"] % 2]
                    cnt["tp"] += 1
                    for kk in range(8):
                        fw.op("pe", lambda e: e.transpose(tp[:, kk, 0:rows], xg[0:rows, kk * 128:(kk + 1) * 128],
                                                          self.ident_bf[0:rows, 0:rows]),
                              reads=[b_xg, self.b_ident], writes=[b_tp])
                    fw.op("act", lambda e: e.activation(out=xT[:, :, k * rows:(k + 1) * rows], in_=tp[:, :, 0:rows], func=AF.Copy),
                          reads=[b_tp], writes=[b_xT])

            def stage_ffn(j):
                ex_i, si = items[j]
                bi, is_ctx = sets[si]
                ix, b_ix, gv, b_gv, nblk, rows = sl[si]
                ncol = nblk * rows
                (wg, b_wg), (wu, b_wu), (wd, b_wd) = wbufs[ex_i % 2]
                dst, b_dst = (self.xcs[bi][:, :], self.b_xcs[bi]) if is_ctx else (self.outs[bi][:, :], self.b_outs[bi])
                grow, b_grow = grows[(bi, is_ctx)]
                xT, b_xT = xgT[j % 3]
                aT, b_aT = actT[j % 2]
                for fc in range(8):
                    ap_, b_ap = a_ps[cnt["au"] % 2]
                    up_, b_up = u_ps[cnt["au"] % 2]
                    sa, b_sa = sas[cnt["au"] % 2]
                    cnt["au"] += 1
                    for kk in range(8):
                        fw.op("pe", lambda e: e.matmul(ap_[:, 0:ncol], lhsT=wg[:, kk, fc * 128:(fc + 1) * 128], rhs=xT[:, kk, 0:ncol],
                                                       start=(kk == 0), stop=(kk == 7)), reads=[b_wg, b_xT], writes=[b_ap])
                    for kk in range(8):
                        fw.op("pe", lambda e: e.matmul(up_[:, 0:ncol], lhsT=wu[:, kk, fc * 128:(fc + 1) * 128], rhs=xT[:, kk, 0:ncol],
                                                       start=(kk == 0), stop=(kk == 7)), reads=[b_wu, b_xT], writes=[b_up])
                    fw.op("act", lambda e: e.activation(out=sa[:, 0:ncol], in_=ap_[:, 0:ncol], func=AF.Silu), reads=[b_ap], writes=[b_sa])
                    fw.op("dve", lambda e: e.tensor_tensor(out=aT[:, fc, 0:ncol], in0=sa[:, 0:ncol], in1=up_[:, 0:ncol], op=ALU.mult),
                          reads=[b_sa, b_up], writes=[b_aT])
                for k in range(nblk):
                    ysc, b_ysc = yscs[cnt["ysc"] % 3]
                    cnt["ysc"] += 1
                    for half in range(2):
                        yp, b_yp = y_ps[cnt["y"] % 2]
                        cnt["y"] += 1
                        for fc in range(8):
                            fw.op("pe", lambda e: e.matmul(yp[0:rows, :], lhsT=aT[:, fc, k * rows:(k + 1) * rows],
                                                           rhs=wd[:, fc, half * 512:(half + 1) * 512],
                                                           start=(fc == 0), stop=(fc == 7)), reads=[b_aT, b_wd], writes=[b_yp])
                        fw.op("dve", lambda e: e.scalar_tensor_tensor(
                            out=ysc[0:rows, half * 512:(half + 1) * 512], in0=yp[0:rows, :], scalar=gv[0:rows, ex_i, k:k + 1],
                            in1=grow[0:rows, half * 512:(half + 1) * 512], op0=ALU.mult, op1=ALU.mult),
                            reads=[b_yp, b_gv, b_grow], writes=[b_ysc])
                    fw.dma("pool", lambda e: e.indirect_dma_start(
                        out=dst, out_offset=bass.IndirectOffsetOnAxis(ap=ix[0:rows, ex_i, k:k + 1], axis=0),
                        in_=ysc[0:rows, :], in_offset=None, compute_op=ALU.add),
                        reads=[b_ysc, b_ix], writes=[b_dst], waw=(k == 0))

            load_w(0)
            stage_gather(0)
            if len(items) > 1:
                stage_gather(1)
            stage_tr(0)
            for j, (ex_i, si) in enumerate(items):
                if si == 0 and ex_i + 1 < NE:
                    load_w(ex_i + 1)
                if j + 2 < len(items):
                    stage_gather(j + 2)
                if j + 1 < len(items):
                    stage_tr(j + 1)
                stage_ffn(j)

    def attn_consts(self, sc):
        fw = self.fw
        bd32, b_bd32 = sc.sb("bd32", [128, 128], F32)
        r32, b_r32 = sc.sb("r32", [128, 128], F32)
        r32b, b_r32b = sc.sb("r32b", [128, 128], F32)
        bd, b_bd = sc.sb("bd", [128, 128], BF16)
        rb, b_rb = sc.sb("rblk", [128, 128], BF16)
        sel, b_sel = sc.sb("sel", [65, 64], F32)
        fw.op("pool", lambda e: e.memset(bd32[:], 0.0), writes=[b_bd32])
        fw.op("pool", lambda e: e.memset(bd32[0:64, 0:64], 1.0), writes=[b_bd32])
        fw.op("pool", lambda e: e.memset(bd32[64:128, 64:128], 1.0), writes=[b_bd32])
        fw.op("pool", lambda e: e.tensor_copy(out=bd[:], in_=bd32[:]), reads=[b_bd32], writes=[b_bd])
        fw.op("pool", lambda e: e.memset(r32[:], -1.0), writes=[b_r32])
        fw.op("pool", lambda e: e.affine_select(out=r32[:], in_=r32[:], pattern=[[-1, 128]], compare_op=ALU.is_equal,
                                                 fill=0.0, base=-32, channel_multiplier=1), reads=[b_r32], writes=[b_r32])
        fw.op("pool", lambda e: e.memset(r32b[:], 1.0), writes=[b_r32b])
        fw.op("pool", lambda e: e.affine_select(out=r32b[:], in_=r32b[:], pattern=[[-1, 128]], compare_op=ALU.is_equal,
                                                 fill=0.0, base=32, channel_multiplier=1), reads=[b_r32b], writes=[b_r32b])
        fw.op("pool", lambda e: e.tensor_tensor(out=r32[:], in0=r32[:], in1=r32b[:], op=ALU.add), reads=[b_r32, b_r32b], writes=[b_r32])
        fw.op("pool", lambda e: e.tensor_tensor(out=r32[:], in0=r32[:], in1=bd32[:], op=ALU.mult), reads=[b_r32, b_bd32], writes=[b_r32])
        fw.op("pool", lambda e: e.tensor_copy(out=rb[:], in_=r32[:]), reads=[b_r32], writes=[b_rb])
        fw.op("pool", lambda e: e.memset(sel[:], 0.0), writes=[b_sel])
        fw.op("pool", lambda e: e.memset(sel[64:65, :], 1.0), writes=[b_sel])
        return (bd, b_bd), (rb, b_rb), (sel, b_sel)

    def phase_attn(self, l, bi):
        fw = self.fw
        j = l // 2
        last = l == DEPTH - 1
        qTd = self.nc.dram_tensor(f"qTd_{l}_{bi}", [128, 8, NT], BF16).ap()
        b_qTd = Buf()
        with fw.scope() as osc:
            kT, b_kT = osc.sb("kT", [128, 2, NT], BF16)
            vA, b_vA = osc.sb("vA", [128, NT // 128, 4, 128], BF16)
            negC, b_negC = osc.sb("negC", [128, 1], F32)
            (bd, b_bd), (rb, b_rb), (sel, b_sel) = self.attn_consts(osc)
            fw.op("pool", lambda e: e.memset(vA[:, :, :, 64:128], 1.0), writes=[b_vA])
            with fw.scope() as sc:
                W = self.norm_work(sc)
                win, b_win = sc.sb("win", [128, 8, 1536], BF16)
                for kk in range(8):
                    for a_ in range(2):
                        for two in range(2):
                            c0 = a_ * 512 + two * 256
                            fw.dma("pool", lambda e: e.dma_start(
                                out=win[:, kk, a_ * 512:(a_ + 1) * 512].rearrange("p (r two d) -> p r two d", r=4, two=2, d=64)[:, :, two, :],
                                in_=self.attn_w_in[j, kk * 128:(kk + 1) * 128, c0:c0 + 256].rearrange("p (r d) -> p r d", r=4, d=64)),
                                writes=[b_win], waw=False)
                for hh in range(2):
                    fw.dma("pool", lambda e: e.dma_start(
                        out=win[:, hh * 4:(hh + 1) * 4, 1024:1536],
                        in_=self.attn_w_in[j, hh * 512:(hh + 1) * 512, 1024:1536].rearrange("(k p) n -> p k n", p=128)),
                        writes=[b_win], waw=False)
                rows = {}
                for r in ((bi, 2) if True else ()):
                    rows[r] = (self.make_arow(sc, l, r, "sc1", self.norm_mix[l:l + 1, :]),
                               self.load_row(sc, "srow", self.mod_row(l, r, "sh1")))
                gq, b_gq = sc.sb("gq", [128, 1], F32)
                gk, b_gk = sc.sb("gk", [128, 1], F32)
                for hf in range(2):
                    fw.dma("sp", lambda e: e.dma_start(out=gq[hf * 64:(hf + 1) * 64, :],
                                                       in_=self.attn_q_norm[j, :].rearrange("(p o) -> p o", o=1)), writes=[b_gq])
                    fw.dma("sp", lambda e: e.dma_start(out=gk[hf * 64:(hf + 1) * 64, :],
                                                       in_=self.attn_k_norm[j, :].rearrange("(p o) -> p o", o=1)), writes=[b_gk])
                gr, b_gr = sc.sb("gr", [128, 2, 64], F32)
                mx, b_mx = sc.sb("mx", [128, 2], F32)
                fw.dma("sp", lambda e: e.dma_start(out=gr[:, 0, :], in_=self.attn_q_norm[j:j + 1, :].partition_broadcast(128)), writes=[b_gr])
                fw.dma("sp", lambda e: e.dma_start(out=gr[:, 1, :], in_=self.attn_k_norm[j:j + 1, :].partition_broadcast(128)), writes=[b_gr])
                fw.op("dve", lambda e: e.tensor_reduce(out=mx[:], in_=gr[:], axis=AX.X, op=ALU.max, apply_absolute_value=True),
                      reads=[b_gr], writes=[b_mx])
                fw.op("dve", lambda e: e.scalar_tensor_tensor(out=negC[:], in0=mx[:, 0:1], scalar=-8.0, in1=mx[:, 1:2],
                                                              op0=ALU.mult, op1=ALU.mult), reads=[b_mx], writes=[b_negC])
                h16s = [sc.sb("h16", [128, D], BF16) for _ in range(2)]
                hTs = [sc.sb("hT", [128, 8, 512], BF16) for _ in range(2)]
                qst = [sc.sb("qst", [128, 8, 512], BF16) for _ in range(2)]
                cs = [sc.sb("cos", [128, 512], F32) for _ in range(2)]
                sn = [sc.sb("sin", [128, 512], F32) for _ in range(2)]
                sqs = [sc.sb("sq", [128, 512], BF16) for _ in range(2)]
                qgs = [sc.sb("qg", [128, 512], BF16) for _ in range(2)]
                lnv, b_lnv = sc.sb("lnv", [128, 512], F32)
                rstds = [sc.sb("rstd", [128, 512], F32) for _ in range(2)]
                t1s = [sc.sb("t1", [128, 512], F32) for _ in range(2)]
                t2s = [sc.sb("t2", [128, 512], F32) for _ in range(2)]
                tpp = [sc.ps("tpp", [128, 8, 128], BF16) for _ in range(2)]
                q_ps = [sc.ps("q_ps", [128, 512]) for _ in range(2)]
                ss_ps, b_ss = sc.ps("ss_ps", [128, 512])
                rq_ps, b_rq = sc.ps("rq_ps", [128, 512])
                v_ps = [sc.ps("v_ps", [128, 256]) for _ in range(2)]
                cnt = dict(t=0, c=0, v=0)
                groups = [(0, 2)] + [(2 + 4 * g, 4) for g in range(8)]
                tiles_all = [t for t0, n in groups for t in range(t0, t0 + n)]
                nxt_x = self.norm_load(W, *self.res_tile(bi, 0))
                ti = 0
                for gi, (t0, nt) in enumerate(groups):
                    is_ctx = gi == 0
                    ntg = nt * 128
                    u0 = t0 * 128
                    (arow, b_arow), (srow, b_srow) = rows[2 if is_ctx else bi]
                    hT, b_hT = hTs[gi % 2]
                    qs, b_qs = qst[gi % 2]
                    if not is_ctx:
                        cg, b_cg = cs[gi % 2]
                        sg, b_sg = sn[gi % 2]
                        l0 = u0 - NCX
                        fw.dma("sp", lambda e: e.dma_start(out=cg[:], in_=self.rope_cos[:, l0:l0 + 512]), writes=[b_cg])
                        fw.dma("sp", lambda e: e.dma_start(out=sg[:], in_=self.rope_sin[:, l0:l0 + 512]), writes=[b_sg])
                    for tt in range(nt):
                        cur_x = nxt_x
                        ti += 1
                        if ti < len(tiles_all):
                            nxt_x = self.norm_load(W, *self.res_tile(bi, tiles_all[ti]))
                        h16, b_h16 = h16s[cnt["t"] % 2]
                        tp, b_tp = tpp[cnt["t"] % 2]
                        cnt["t"] += 1
                        self.norm_tile(W, cur_x, arow, b_arow, srow, b_srow, h16[:], b_h16)
                        for kk in range(8):
                            fw.op("pe", lambda e: e.transpose(tp[:, kk, :], h16[:, kk * 128:(kk + 1) * 128], self.ident_bf[:]),
                                  reads=[b_h16, self.b_ident], writes=[b_tp])
                        fw.op("act", lambda e: e.activation(out=hT[:, :, tt * 128:(tt + 1) * 128], in_=tp[:], func=AF.Copy),
                              reads=[b_tp], writes=[b_hT])
                        vp, b_vp = v_ps[cnt["v"] % 2]
                        cnt["v"] += 1
                        for kk in range(8):
                            fw.op("pe", lambda e: e.matmul(vp[:], lhsT=hT[:, kk, tt * 128:(tt + 1) * 128], rhs=win[:, kk, 1280:1536],
                                                           start=(kk == 0), stop=(kk == 7)), reads=[b_hT, b_win], writes=[b_vp])
                        fw.op("dve", lambda e: e.tensor_copy(out=vA[:, t0 + tt, :, 0:64], in_=vp[:].rearrange("p (g d) -> p g d", g=4)),
                              reads=[b_vp], writes=[b_vA])
                    for c in range(10):
                        qp, b_qp = q_ps[cnt["c"] % 2]
                        sq, b_sq = sqs[cnt["c"] % 2]
                        qg, b_qg = qgs[cnt["c"] % 2]
                        rstd, b_rstd = rstds[cnt["c"] % 2]
                        t1, b_t1 = t1s[cnt["c"] % 2]
                        t2, b_t2 = t2s[cnt["c"] % 2]
                        cnt["c"] += 1
                        is_q = c < 8
                        for kk in range(8):
                            lw = win[:, kk, c * 128:(c + 1) * 128]
                            fw.op("pe", lambda e: e.matmul(qp[:, 0:ntg], lhsT=lw, rhs=hT[:, kk, 0:ntg], start=(kk == 0), stop=(kk == 7)),
                                  reads=[b_win, b_hT], writes=[b_qp])
                        gcol, b_gcol = (gq, b_gq) if is_q else (gk, b_gk)
                        fw.op("act", lambda e: e.activation(out=sq[:, 0:ntg], in_=qp[:, 0:ntg], func=AF.Square), reads=[b_qp], writes=[b_sq])
                        fw.op("act", lambda e: e.activation(out=qg[:, 0:ntg], in_=qp[:, 0:ntg], func=AF.Copy, scale=gcol[:, 0:1]),
                              reads=[b_qp, b_gcol], writes=[b_qg])
                        fw.op("pe", lambda e: e.matmul(ss_ps[:, 0:ntg], lhsT=bd[:], rhs=sq[:, 0:ntg], start=True, stop=True),
                              reads=[b_bd, b_sq], writes=[b_ss])
                        if not is_ctx:
                            fw.op("pe", lambda e: e.matmul(rq_ps[:, 0:ntg], lhsT=rb[:], rhs=qg[:, 0:ntg], start=True, stop=True),
                                  reads=[b_rb, b_qg], writes=[b_rq])
                        fw.op("act", lambda e: e.activation(out=lnv[:, 0:ntg], in_=ss_ps[:, 0:ntg], func=AF.Ln, scale=1.0 / 64, bias=EPS),
                              reads=[b_ss], writes=[b_lnv])
                        fw.op("act", lambda e: e.activation(out=rstd[:, 0:ntg], in_=lnv[:, 0:ntg], func=AF.Exp, scale=-0.5),
                              reads=[b_lnv], writes=[b_rstd])
                        if is_q:
                            dst, b_dst = qs[:, c, 0:ntg], b_qs
                        else:
                            dst, b_dst = kT[:, c - 8, u0:u0 + ntg], b_kT
                        if is_ctx:
                            fw.op("dve", lambda e: e.tensor_tensor(out=dst, in0=qg[:, 0:ntg], in1=rstd[:, 0:ntg], op=ALU.mult),
                                  reads=[b_qg, b_rstd], writes=[b_dst])
                        else:
                            fw.op("dve", lambda e: e.tensor_tensor(out=t1[:], in0=qg[:], in1=cg[:], op=ALU.mult),
                                  reads=[b_qg, b_cg], writes=[b_t1])
                            fw.op("dve", lambda e: e.tensor_tensor(out=t2[:], in0=rq_ps[:], in1=sg[:], op=ALU.mult),
                                  reads=[b_rq, b_sg], writes=[b_t2])
                            fw.op("pool", lambda e: e.tensor_tensor(out=t1[:], in0=t1[:], in1=t2[:], op=ALU.add),
                                  reads=[b_t1, b_t2], writes=[b_t1])
                            fw.op("dve", lambda e: e.tensor_tensor(out=dst, in0=t1[:], in1=rstd[:], op=ALU.mult),
                                  reads=[b_t1, b_rstd], writes=[b_dst])
                    fw.dma("sp", lambda e: e.dma_start(out=qTd[:, :, u0:u0 + ntg], in_=qs[:, :, 0:ntg]), reads=[b_qs], writes=[b_qTd], waw=False)
            with fw.scope() as sc:
                wo, b_wo = sc.sb("wo", [64, 16, D], BF16)
                for hh in range(2):
                    fw.dma("pool", lambda e: e.dma_start(
                        out=wo[:, hh * 8:(hh + 1) * 8, :],
                        in_=self.attn_w_out[j, hh * 512:(hh + 1) * 512, :].rearrange("(h p) n -> p h n", p=64)),
                        writes=[b_wo], waw=False)
                g1 = {bi: self.load_row(sc, "g1row", self.mod_row(l, bi, "g1"))}
                if not last:
                    g1[2] = self.load_row(sc, "g1rowc", self.mod_row(l, 2, "g1"))
                qts = [sc.sb("qt", [128, 16, 128], BF16) for _ in range(3)]
                for qt_, b_qt_ in qts:
                    fw.op("pool", lambda e: e.memset(qt_[:], 0.0), writes=[b_qt_])
                pTs = [sc.sb("pT", [128, 1024], BF16) for _ in range(4)]
                osbs = [sc.sb("osb", [65, 512], F32) for _ in range(2)]
                rdens = [sc.sb("rden", [64, 512], F32) for _ in range(2)]
                oTns = [sc.sb("oTn", [64, 4, 512], BF16) for _ in range(2)]
                xts = [sc.sb("xres", [128, D], F32) for _ in range(3)]
                tmps = [sc.sb("xtmp", [128, D], F32) for _ in range(2)]
                s_ps = [sc.ps("s_ps", [128, 1024]) for _ in range(2)]
                o_ps = [sc.ps("o_ps", [128, 512]) for _ in range(2)]
                den_ps, b_den = sc.ps("den_ps", [64, 512])
                y_ps, b_yp = sc.ps("y_ps", [128, 512])
                qtiles = ([] if last else [0, 1]) + list(range(2, NT // 128))
                LOOK = 2
                units = []
                for qi, t in enumerate(qtiles):
                    npair = (2 if t < 2 else NT // 128) // 2
                    for g in range(4):
                        for kp in range(npair):
                            units.append((qi, t, g, kp, npair))
                qstate = {}

                def q_begin(qi, t):
                    qt, b_qt = qts[qi % 3]
                    xt, b_xt = xts[qi % 3]
                    for g_ in range(4):
                        hf_, c0_ = g_ % 2, (g_ // 2) * 4
                        fw.dma("sp", lambda e: e.dma_start(out=qt[hf_ * 64:(hf_ + 1) * 64, g_ * 4:(g_ + 1) * 4, :],
                                                           in_=qTd[hf_ * 64:(hf_ + 1) * 64, c0_:c0_ + 4, t * 128:(t + 1) * 128]),
                               reads=[b_qTd], writes=[b_qt])
                    rap, b_rap = self.res_tile(bi, t)
                    fw.dma("sp", lambda e: e.dma_start(out=xt[:], in_=rap), reads=[b_rap], writes=[b_xt])
                    qstate[qi] = (qt, b_qt, xt, b_xt, rap, b_rap)

                def issue_qk(ui):
                    qi, t, g, kp, npair = units[ui]
                    if qi not in qstate:
                        q_begin(qi, t)
                    qt, b_qt = qstate[qi][0:2]
                    hf, cq = g % 2, (g // 2) * 4
                    sp_, b_sp = s_ps[ui % 2]
                    pT, b_pT = pTs[ui % 4]
                    for i2 in range(2):
                        kb = kp * 2 + i2
                        fw.op("pe", lambda e: e.matmul(sp_[:, i2 * 512:(i2 + 1) * 512],
                                                       lhsT=kT[:, g // 2, kb * 128:(kb + 1) * 128],
                                                       rhs=qt[:, g * 4:(g + 1) * 4, :], start=True, stop=True),
                              reads=[b_kT, b_qt], writes=[b_sp])
                    fw.op("act", lambda e: e.activation(out=pT[:], in_=sp_[:], func=AF.Exp, scale=0.125, bias=negC[:, 0:1]),
                          reads=[b_sp, b_negC], writes=[b_pT])

                pending = []

                def fin1(qi, g, oi):
                    op_, b_op = o_ps[oi % 2]
                    osb, b_osb = osbs[oi % 2]
                    fw.op("dve", lambda e: e.tensor_copy(out=osb[:], in_=op_[0:65, :]), reads=[b_op], writes=[b_osb])

                def fin2(qi, g, oi):
                    osb, b_osb = osbs[oi % 2]
                    rden, b_rden = rdens[oi % 2]
                    oTn, b_oTn = oTns[qi % 2]
                    fw.op("pe", lambda e: e.matmul(den_ps[:], lhsT=sel[:], rhs=osb[:], start=True, stop=True),
                          reads=[b_sel, b_osb], writes=[b_den])
                    fw.op("dve", lambda e: e.reciprocal(out=rden[:], in_=den_ps[:]), reads=[b_den], writes=[b_rden])
                    fw.op("dve", lambda e: e.tensor_tensor(out=oTn[:, g, :], in0=osb[0:64, :], in1=rden[:], op=ALU.mult),
                          reads=[b_osb, b_rden], writes=[b_oTn])

                def fin3(qi, t):
                    qt, b_qt, xt, b_xt, rap, b_rap = qstate.pop(qi)
                    oTn, b_oTn = oTns[qi % 2]
                    tmp, b_tmp = tmps[qi % 2]
                    grow, b_grow = g1[2 if t < 2 else bi]
                    for half in range(2):
                        for h in range(16):
                            g, r_ = h // 4, h % 4
                            fw.op("pe", lambda e: e.matmul(y_ps[:], lhsT=oTn[:, g, r_ * 128:(r_ + 1) * 128],
                                                           rhs=wo[:, h, half * 512:(half + 1) * 512], start=(h == 0), stop=(h == 15)),
                                  reads=[b_oTn, b_wo], writes=[b_yp])
                        hs = slice(half * 512, (half + 1) * 512)
                        fw.op("dve", lambda e: e.tensor_tensor(out=tmp[:, hs], in0=y_ps[:], in1=grow[:, hs], op=ALU.mult),
                              reads=[b_yp, b_grow], writes=[b_tmp])
                        fw.op("pool", lambda e: e.tensor_tensor(out=tmp[:, hs], in0=tmp[:, hs], in1=xt[:, hs], op=ALU.add),
                              reads=[b_tmp, b_xt], writes=[b_tmp])
                    fw.dma("sp", lambda e: e.dma_start(out=rap, in_=tmp[:]), reads=[b_tmp], writes=[b_rap], waw=False)

                ocount = 0
                for ui in range(min(LOOK, len(units))):
                    issue_qk(ui)
                for ui, (qi, t, g, kp, npair) in enumerate(units):
                    if ui + LOOK < len(units):
                        issue_qk(ui + LOOK)
                    due = [p for p in pending if p[0] <= ui]
                    pending[:] = [p for p in pending if p[0] > ui]
                    for _, fn in due:
                        fn()
                    pT, b_pT = pTs[ui % 4]
                    op_, b_op = o_ps[ocount % 2]
                    for i2 in range(2):
                        kb = kp * 2 + i2
                        fw.op("pe", lambda e: e.matmul(op_[:, :], lhsT=vA[:, kb, g, :], rhs=pT[:, i2 * 512:(i2 + 1) * 512],
                                                       start=(kb == 0), stop=(kb == 2 * npair - 1)),
                              reads=[b_vA, b_pT], writes=[b_op])
                    if kp == npair - 1:
                        oi = ocount
                        ocount += 1
                        fin1(qi, g, oi)
                        pending.append((ui + 2, (lambda qi=qi, g=g, oi=oi: fin2(qi, g, oi))))
                        if g == 3:
                            pending.append((ui + 4, (lambda qi=qi, t=t: fin3(qi, t))))
                for _, fn in pending:
                    fn()

    def phase_mlstm(self, l, bi):
        fw = self.fw
        nc = self.nc
        j = l // 2
        NCH = NT // 64
        qk_d = nc.dram_tensor(f"qk_d_{l}_{bi}", [NCH, 128, 8, 64], BF16).ap()
        oT_d = nc.dram_tensor(f"oT_d_{l}_{bi}", [NT // 128, 128, 8, 128], BF16).ap()
        v_d = nc.dram_tensor(f"v_d_{l}_{bi}", [NT, D], BF16).ap()
        hf_d = nc.dram_tensor(f"hf_d_{l}_{bi}", [NT, D], F32).ap()
        b_qk, b_oT, b_v, b_hf = Buf(), Buf(), Buf(), Buf()
        order = {0: list(range(NCH)), 1: [3, 2, 1, 0] + list(range(NCH - 1, 3, -1))}
        with fw.scope() as osc:
            tokT, b_tokT = osc.sb("tokT", [64, NCH, 16], F32)
            WIbc, b_WIbc = osc.sb("WIbc", [128, 8, NCH], F32)
            with fw.scope() as gsc:
                IG, b_IG = gsc.sb("IG", [8, NT], F32)
                FG, b_FG = gsc.sb("FG", [8, NT], F32)
                with fw.scope() as sc:
                    W = self.norm_work(sc)
                    win, b_win = sc.sb("win", [128, 8, 3072], BF16)
                    wgp, b_wgp = sc.sb("wgp", [128, 8, 16], BF16)
                    for hh in range(2):
                        for cc in range(2):
                            fw.dma("pool", lambda e: e.dma_start(
                                out=win[:, hh * 4:(hh + 1) * 4, cc * 1536:(cc + 1) * 1536],
                                in_=self.mlstm_w_in[j, hh * 512:(hh + 1) * 512, cc * 1536:(cc + 1) * 1536].rearrange("(k p) n -> p k n", p=128)),
                                writes=[b_win], waw=False)
                    for dst0, src0 in ((0, 0), (4, 8), (8, 4), (12, 12)):
                        fw.dma("pool", lambda e: e.dma_start(
                            out=wgp[:, :, dst0:dst0 + 4],
                            in_=self.mlstm_w_in[j, :, 3072 + src0:3072 + src0 + 4].rearrange("(k p) n -> p k n", p=128)),
                            writes=[b_wgp], waw=False)
                    bg, b_bg = sc.sb("bg", [8, 2], F32)
                    for (prow, src0, col) in ((0, 0, 0), (4, 8, 0), (0, 4, 1), (4, 12, 1)):
                        fw.dma("sp", lambda e: e.dma_start(out=bg[prow:prow + 4, col:col + 1],
                                                           in_=self.mlstm_b_gate[j, src0:src0 + 4].rearrange("(p o) -> p o", o=1)),
                               writes=[b_bg])
                    fw.op("dve", lambda e: e.tensor_scalar(out=bg[:], in0=bg[:], scalar1=1.0 / 15.0, scalar2=None, op0=ALU.mult),
                          reads=[b_bg], writes=[b_bg])
                    nwrow = self.load_row(sc, "nwrow", self.norm_mix[l:l + 1, :])
                    rows = {}
                    for r in (bi, 2):
                        ar, b_ar = self.load_row(sc, "arow", self.mod_row(l, r, "sc1"))
                        fw.op("dve", lambda e: e.scalar_tensor_tensor(out=ar[:], in0=ar[:], scalar=1.0, in1=nwrow[0][:],
                                                                      op0=ALU.add, op1=ALU.mult), reads=[b_ar, nwrow[1]], writes=[b_ar])
                        rows[r] = ((ar, b_ar), self.load_row(sc, "srow", self.mod_row(l, r, "sh1")))
                    h16s = [sc.sb("h16", [128, D], BF16) for _ in range(2)]
                    hTs = [sc.sb("hT", [128, 8, 512], BF16) for _ in range(2)]
                    qkst, b_qkst = sc.sb("qkst", [128, 8, 512], BF16)
                    ost, b_ost = sc.sb("ost", [128, 8, 512], BF16)
                    vsts = [sc.sb("vst", [128, D], BF16) for _ in range(2)]
                    tpp = [sc.ps("tpp", [128, 8, 128], BF16) for _ in range(2)]
                    c_ps = [sc.ps("c_ps", [128, 512]) for _ in range(2)]
                    g_ps = [sc.ps("g_ps", [128, 512]) for _ in range(2)]
                    v_ps = [sc.ps("v_ps", [128, 512]) for _ in range(2)]
                    cnt = dict(t=0, c=0, v=0, vs=0)
                    groups = [(0, 2)] + [(2 + 4 * g, 4) for g in range(8)]
                    tiles_all = [t for t0, n in groups for t in range(t0, t0 + n)]
                    nxt_x = self.norm_load(W, *self.res_tile(bi, 0))
                    ti = 0
                    for gi, (t0, nt) in enumerate(groups):
                        is_ctx = gi == 0
                        ntg = nt * 128
                        u0 = t0 * 128
                        (arow, b_arow), (srow, b_srow) = rows[2 if is_ctx else bi]
                        hT, b_hT = hTs[gi % 2]
                        for tt in range(nt):
                            cur_x = nxt_x
                            ti += 1
                            if ti < len(tiles_all):
                                nxt_x = self.norm_load(W, *self.res_tile(bi, tiles_all[ti]))
                            h16, b_h16 = h16s[cnt["t"] % 2]
                            tp, b_tp = tpp[cnt["t"] % 2]
                            cnt["t"] += 1
                            self.norm_tile(W, cur_x, arow, b_arow, srow, b_srow, h16[:], b_h16)
                            for kk in range(8):
                                fw.op("pe", lambda e: e.transpose(tp[:, kk, :], h16[:, kk * 128:(kk + 1) * 128], self.ident_bf[:]),
                                      reads=[b_h16, self.b_ident], writes=[b_tp])
                            fw.op("act", lambda e: e.activation(out=hT[:, :, tt * 128:(tt + 1) * 128], in_=tp[:], func=AF.Copy),
                                  reads=[b_tp], writes=[b_hT])
                            vst, b_vst = vsts[cnt["vs"] % 2]
                            cnt["vs"] += 1
                            for half in range(2):
                                vp, b_vp = v_ps[cnt["v"] % 2]
                                cnt["v"] += 1
                                for kk in range(8):
                                    fw.op("pe", lambda e: e.matmul(vp[:], lhsT=hT[:, kk, tt * 128:(tt + 1) * 128],
                                                                   rhs=win[:, kk, 1024 + half * 512:1536 + half * 512],
                                                                   start=(kk == 0), stop=(kk == 7)), reads=[b_hT, b_win], writes=[b_vp])
                                fw.op("dve", lambda e: e.tensor_copy(out=vst[:, half * 512:(half + 1) * 512], in_=vp[:]),
                                      reads=[b_vp], writes=[b_vst])
                            ut = (t0 + tt) * 128
                            fw.dma("sp", lambda e: e.dma_start(out=v_d[ut:ut + 128, :], in_=vst[:]), reads=[b_vst], writes=[b_v], waw=False)
                        for c in range(16):
                            cp, b_cp = c_ps[cnt["c"] % 2]
                            cnt["c"] += 1
                            col0 = c * 128 if c < 8 else 2048 + (c - 8) * 128
                            for kk in range(8):
                                fw.op("pe", lambda e: e.matmul(cp[:, 0:ntg], lhsT=win[:, kk, col0:col0 + 128], rhs=hT[:, kk, 0:ntg],
                                                               start=(kk == 0), stop=(kk == 7)), reads=[b_win, b_hT], writes=[b_cp])
                            if c < 4:
                                fw.op("act", lambda e: e.activation(out=qkst[:, c, 0:ntg], in_=cp[:, 0:ntg], func=AF.Copy, scale=128 ** -0.5),
                                      reads=[b_cp], writes=[b_qkst])
                            elif c < 8:
                                fw.op("dve", lambda e: e.tensor_copy(out=qkst[:, c, 0:ntg], in_=cp[:, 0:ntg]), reads=[b_cp], writes=[b_qkst])
                            else:
                                fw.op("act", lambda e: e.activation(out=ost[:, c - 8, 0:ntg], in_=cp[:, 0:ntg], func=AF.Sigmoid),
                                      reads=[b_cp], writes=[b_ost])
                        for cc in range(ntg // 64):
                            ch = u0 // 64 + cc
                            fw.dma("sp", lambda e: e.dma_start(out=qk_d[ch], in_=qkst[:, :, cc * 64:(cc + 1) * 64]),
                                   reads=[b_qkst], writes=[b_qk], waw=False)
                        for tt in range(nt):
                            fw.dma("sp", lambda e: e.dma_start(out=oT_d[t0 + tt], in_=ost[:, :, tt * 128:(tt + 1) * 128]),
                                   reads=[b_ost], writes=[b_oT], waw=False)
                        for gt, (dst, b_dst) in enumerate(((IG, b_IG), (FG, b_FG))):
                            gp, b_gp = g_ps[gt]
                            for kk in range(8):
                                fw.op("pe", lambda e: e.matmul(gp[0:8, 0:ntg], lhsT=wgp[:, kk, gt * 8:(gt + 1) * 8], rhs=hT[:, kk, 0:ntg],
                                                               start=(kk == 0), stop=(kk == 7)), reads=[b_wgp, b_hT], writes=[b_gp])
                            fw.op("act", lambda e: e.activation(out=dst[:, u0:u0 + ntg], in_=gp[0:8, 0:ntg], func=AF.Tanh,
                                                                scale=1.0 / 15.0, bias=bg[:, gt:gt + 1]),
                                  reads=[b_gp, b_bg], writes=[b_dst])
                with fw.scope() as sc:
                    X2, b_X2 = sc.sb("X2", [8, NT], F32)
                    X3, b_X3 = sc.sb("X3", [8, NT], F32)
                    msk, b_msk = sc.sb("msk", [8, NT], F32)
                    dm, b_dm = sc.sb("dm", [8, 2], F32)
                    sm = {k: sc.sb(k, [8, NCH], F32) for k in ("amax", "blast", "mn0", "mn1", "mb0", "mb1", "mnext", "mbef", "Mp", "wi")}
                    rbd, b_rbd = sc.sb("rbd", [8, 8, NCH], F32)
                    ones8, b_ones8 = sc.sb("ones8", [8, 128], F32)
                    zero1, b_zero1 = sc.sb("zero1", [8, 1], F32)
                    tk_ps = [sc.ps("tk_ps", [64, 32, 16], F32) for _ in range(2)]
                    wi_ps = [sc.ps("wi_ps", [128, 512], F32) for _ in range(2)]
                    v3 = lambda t: t[:].rearrange("p (c s) -> p c s", s=64)
                    fw.op("pool", lambda e: e.memset(msk[:], 1.0), writes=[b_msk])
                    fw.op("pool", lambda e: e.memset(v3(msk)[:, :, 0:1], 0.0), writes=[b_msk])
                    fw.op("pool", lambda e: e.memset(ones8[:], 1.0), writes=[b_ones8])
                    fw.op("pool", lambda e: e.memset(zero1[:], 0.0), writes=[b_zero1])
                    fw.op("pool", lambda e: e.iota(dm[:, 0:1], pattern=[[0, 1]], base=0, channel_multiplier=1,
                                                   allow_small_or_imprecise_dtypes=True), writes=[b_dm])
                    fw.op("dve", lambda e: e.tensor_single_scalar(out=dm[:, 1:2], in_=dm[:, 0:1], scalar=3.5, op=ALU.is_ge),
                          reads=[b_dm], writes=[b_dm])
                    fw.op("act", lambda e: e.activation(out=FG[:], in_=FG[:], func=AF.Exp, scale=-15.0), reads=[b_FG], writes=[b_FG])
                    fw.op("act", lambda e: e.activation(out=FG[:], in_=FG[:], func=AF.Ln, bias=1.0), reads=[b_FG], writes=[b_FG])
                    fw.op("dve", lambda e: e.tensor_tensor_scan(out=X2[:], data0=msk[:], data1=FG[:], initial=0.0, op0=ALU.mult, op1=ALU.add),
                          reads=[b_msk, b_FG], writes=[b_X2])
                    tot = v3(X2)[:, :, 63:64]
                    fw.op("dve", lambda e: e.tensor_tensor(out=X3[:], in0=FG[:], in1=X2[:], op=ALU.subtract), reads=[b_FG, b_X2], writes=[b_X3])
                    fw.op("dve", lambda e: e.tensor_tensor(out=v3(X3), in0=v3(X3), in1=tot.to_broadcast([8, NCH, 64]), op=ALU.add),
                          reads=[b_X3, b_X2], writes=[b_X3])
                    fw.op("dve", lambda e: e.tensor_tensor(out=X3[:], in0=X3[:], in1=X2[:], op=ALU.subtract), reads=[b_X3, b_X2], writes=[b_X3])
                    fw.op("dve", lambda e: e.tensor_scalar(out=sm["blast"][0][:], in0=v3(X2)[:, :, 63], scalar1=-1.0, scalar2=None, op0=ALU.mult),
                          reads=[b_X2], writes=[sm["blast"][1]])
                    NB, b_NB = FG, b_FG
                    fw.op("dve", lambda e: e.scalar_tensor_tensor(out=NB[:], in0=X3[:], scalar=dm[:, 1:2], in1=X2[:], op0=ALU.mult, op1=ALU.add),
                          reads=[b_X3, b_dm, b_X2], writes=[b_NB])
                    fw.op("dve", lambda e: e.scalar_tensor_tensor(out=IG[:], in0=IG[:], scalar=15.0, in1=NB[:], op0=ALU.mult, op1=ALU.add),
                          reads=[b_IG, b_NB], writes=[b_IG])
                    amax, b_amax = sm["amax"]
                    blast, b_blast = sm["blast"]
                    fw.op("dve", lambda e: e.tensor_reduce(out=amax[:], in_=v3(IG), axis=AX.X, op=ALU.max), reads=[b_IG], writes=[b_amax])
                    for d in range(2):
                        mn, b_mn = sm[f"mn{d}"]
                        mb, b_mb = sm[f"mb{d}"]
                        prev = None
                        for c in order[d]:
                            sc_ap = zero1[:, 0:1] if prev is None else mn[:, prev:prev + 1]
                            fw.op("dve", lambda e: e.tensor_copy(out=mb[:, c:c + 1], in_=sc_ap), reads=[b_mn, b_zero1], writes=[b_mb])
                            fw.op("dve", lambda e: e.scalar_tensor_tensor(out=mn[:, c:c + 1], in0=amax[:, c:c + 1], scalar=sc_ap,
                                                                          in1=blast[:, c:c + 1], op0=ALU.max, op1=ALU.add),
                                  reads=[b_amax, b_blast, b_mn, b_zero1], writes=[b_mn])
                            prev = c
                    for nm, (a0, a1) in (("mnext", ("mn0", "mn1")), ("mbef", ("mb0", "mb1"))):
                        o_, b_o = sm[nm]
                        fw.op("dve", lambda e: e.tensor_tensor(out=o_[:], in0=sm[a1][0][:], in1=sm[a0][0][:], op=ALU.subtract),
                              reads=[sm[a0][1], sm[a1][1]], writes=[b_o])
                        fw.op("dve", lambda e: e.scalar_tensor_tensor(out=o_[:], in0=o_[:], scalar=dm[:, 1:2], in1=sm[a0][0][:],
                                                                      op0=ALU.mult, op1=ALU.add), reads=[b_o, b_dm, sm[a0][1]], writes=[b_o])
                    Mp, b_Mp = sm["Mp"]
                    wi, b_wi = sm["wi"]
                    fw.op("dve", lambda e: e.tensor_tensor(out=Mp[:], in0=sm["mnext"][0][:], in1=blast[:], op=ALU.subtract),
                          reads=[sm["mnext"][1], b_blast], writes=[b_Mp])
                    fw.op("dve", lambda e: e.tensor_tensor(out=wi[:], in0=sm["mbef"][0][:], in1=Mp[:], op=ALU.subtract),
                          reads=[sm["mbef"][1], b_Mp], writes=[b_wi])
                    fw.op("act", lambda e: e.activation(out=wi[:], in_=wi[:], func=AF.Exp), reads=[b_wi], writes=[b_wi])
                    mpb = Mp[:].unsqueeze(2).to_broadcast([8, NCH, 64])
                    fw.op("dve", lambda e: e.tensor_tensor(out=v3(IG), in0=v3(IG), in1=mpb, op=ALU.subtract), reads=[b_IG, b_Mp], writes=[b_IG])
                    fw.op("act", lambda e: e.activation(out=IG[:], in_=IG[:], func=AF.Exp), reads=[b_IG], writes=[b_IG])
                    fw.op("dve", lambda e: e.tensor_tensor(out=v3(NB), in0=v3(NB), in1=mpb, op=ALU.subtract), reads=[b_NB, b_Mp], writes=[b_NB])
                    fw.op("act", lambda e: e.activation(out=NB[:], in_=NB[:], func=AF.Exp), reads=[b_NB], writes=[b_NB])
                    for blk in range(3):
                        tk, b_tk = tk_ps[blk % 2]
                        c0 = blk * 32
                        nchb = min(32, NCH - c0)
                        for cc in range(nchb):
                            c = c0 + cc
                            fw.op("pe", lambda e: e.transpose(tk[:, cc, 0:8], IG[:, c * 64:(c + 1) * 64], self.ident_f[0:8, 0:8]),
                                  reads=[b_IG, self.b_ident], writes=[b_tk])
                            fw.op("pe", lambda e: e.transpose(tk[:, cc, 8:16], NB[:, c * 64:(c + 1) * 64], self.ident_f[0:8, 0:8]),
                                  reads=[b_NB, self.b_ident], writes=[b_tk])
                        fw.op("dve", lambda e: e.tensor_copy(out=tokT[:, c0:c0 + nchb, :], in_=tk[:, 0:nchb, :]), reads=[b_tk], writes=[b_tokT])
                    fw.op("dve", lambda e: e.tensor_tensor(out=rbd[:], in0=wi[:].unsqueeze(1).to_broadcast([8, 8, NCH]),
                                                           in1=self.ident_f[0:8, 0:8].unsqueeze(2).to_broadcast([8, 8, NCH]), op=ALU.mult),
                          reads=[b_wi, self.b_ident], writes=[b_rbd])
                    rflat = rbd[:].rearrange("p a c -> p (a c)")
                    wflat = WIbc[:].rearrange("p a c -> p (a c)")
                    for i, (n0, n1) in enumerate(((0, 512), (512, 8 * NCH))):
                        wp, b_wp = wi_ps[i]
                        fw.op("pe", lambda e: e.matmul(wp[:, 0:n1 - n0], lhsT=ones8[:], rhs=rflat[:, n0:n1], start=True, stop=True),
                              reads=[b_ones8, b_rbd], writes=[b_wp])
                        fw.op("dve", lambda e: e.tensor_copy(out=wflat[:, n0:n1], in_=wp[:, 0:n1 - n0]), reads=[b_wp], writes=[b_WIbc])
            with fw.scope() as sc:
                masks = []
                for d, sgn in ((0, -1), (1, 1)):
                    m_, b_m = sc.sb("cmask", [64, 64], F32)
                    fw.op("pool", lambda e: e.memset(m_[:], 1.0), writes=[b_m])
                    fw.op("pool", lambda e: e.affine_select(out=m_[:], in_=m_[:], pattern=[[-sgn, 64]], compare_op=ALU.is_ge, fill=0.0,
                                                             base=0, channel_multiplier=sgn), reads=[b_m], writes=[b_m])
                    masks.append((m_, b_m))
                wout, b_wout = sc.sb("wout", [128, 8, D], BF16)
                for hh in range(2):
                    fw.dma("pool", lambda e: e.dma_start(
                        out=wout[:, hh * 4:(hh + 1) * 4, :],
                        in_=self.mlstm_w_out[j, hh * 512:(hh + 1) * 512, :].rearrange("(k p) n -> p k n", p=128)),
                        writes=[b_wout], waw=False)
                WN, b_WN = self.load_row(sc, "wnrow", self.mlstm_norm[j:j + 1, :])
                g1 = {bi: self.load_row(sc, "g1row", self.mod_row(l, bi, "g1")), 2: self.load_row(sc, "g1rowc", self.mod_row(l, 2, "g1"))}
                qkcs = [sc.sb("qkc", [128, 8, 64], BF16) for _ in range(3)]
                vchs = [sc.sb("vch", [64, 4, 257], BF16) for _ in range(3)]
                for vch, b_vch in vchs:
                    fw.op("pool", lambda e: e.memset(vch[:, :, 256:257], 1.0), writes=[b_vch])
                kws = [sc.sb("kw", [64, 4, 128], BF16) for _ in range(2)]
                ptms = [sc.sb("ptm", [64, 4, 64], BF16) for _ in range(2)]
                ptf, b_ptf = sc.sb("ptf", [64, 4, 64], F32)
                Cst = [sc.sb("Cst", [128, 257], F32) for _ in range(4)]
                Dst = [sc.sb("Dst", [128, 257], BF16) for _ in range(4)]
                dds = [sc.sb("dd", [64, 2], F32) for _ in range(4)]
                hchs = [sc.sb("hch", [64, 4, 256], F32) for _ in range(2)]
                hfchs = [sc.sb("hfch", [64, 4, 256], F32) for _ in range(2)]
                sqt, b_sqt = sc.sb("sqt", [64, 4, 256], BF16)
                rss = [sc.sb("rs", [64, 12], F32) for _ in range(2)]
                aws = [sc.sb("aw", [64, D], F32) for _ in range(2)]
                pending_ro = [None]
                a16s = [sc.sb("a16", [64, D], BF16) for _ in range(2)]
                aTs = [sc.sb("aT", [128, 8, 128], BF16) for _ in range(2)]
                oTts = [sc.sb("oTt", [128, 8, 128], BF16) for _ in range(2)]
                xts = [sc.sb("xres", [128, D], F32) for _ in range(2)]
                tmps = [sc.sb("xtmp", [128, D], F32) for _ in range(2)]
                pt_ps, b_ptps = sc.ps("pt_ps", [128, 512])
                o_ps = [sc.ps("o_ps", [128, 512]) for _ in range(2)]
                u_ps = [sc.ps("u_ps", [128, 512]) for _ in range(2)]
                kw_ps, b_kwps = sc.ps("kw_ps", [64, 4, 128], BF16)
                aT_ps, b_aTps = sc.ps("aT_ps", [128, 8, 128], BF16)
                y_ps, b_yps = sc.ps("y_ps", [128, 512])
                cnt = dict(q=0, pt=0, o=0, u=0, h=0, t=0)
                for d in range(2):
                    cm, b_cm = masks[d]
                    for h in range(4):
                        fw.op("pool", lambda e: e.memset(Cst[h][0][:], 0.0), writes=[Cst[h][1]])
                        fw.op("pool", lambda e: e.memset(Dst[h][0][:], 0.0), writes=[Dst[h][1]])
                    ordr = order[d]

                    def load_chunk(si):
                        c = ordr[si]
                        qkc, b_qkc = qkcs[cnt["q"] % 3]
                        vch, b_vch = vchs[cnt["q"] % 3]
                        cnt["q"] += 1
                        fw.dma("sp", lambda e: e.dma_start(out=qkc[:], in_=qk_d[c]), reads=[b_qk], writes=[b_qkc])
                        fw.dma("sp", lambda e: e.dma_start(out=vch[:, :, 0:256], in_=v_d[c * 64:(c + 1) * 64, :].rearrange("p (h d) -> p h d", h=4)),
                               reads=[b_v], writes=[b_vch])
                        return (qkc, b_qkc, vch, b_vch)
                    nxt = load_chunk(0)
                    for si, c in enumerate(ordr):
                        qkc, b_qkc, vch, b_vch = nxt
                        if si + 1 < NCH:
                            nxt = load_chunk(si + 1)
                        kw, b_kw = kws[si % 2]
                        ptm, b_ptm = ptms[si % 2]
                        for h in range(4):
                            fw.op("pe", lambda e: e.transpose(kw_ps[:, h, :], qkc[:, 4 + h, :], self.ident_bf[:]),
                                  reads=[b_qkc, self.b_ident], writes=[b_kwps])
                        for h in range(4):
                            fw.op("pe", lambda e: e.matmul(pt_ps[0:64, h * 64:(h + 1) * 64], lhsT=qkc[:, 4 + h, :], rhs=qkc[:, h, :],
                                                           start=True, stop=True), reads=[b_qkc], writes=[b_ptps])
                        wk4 = tokT[:, c, d * 4:d * 4 + 4].unsqueeze(2)
                        fw.op("dve", lambda e: e.tensor_tensor(out=kw[:], in0=kw_ps[:], in1=wk4.to_broadcast([64, 4, 128]), op=ALU.mult),
                              reads=[b_kwps, b_tokT], writes=[b_kw])
                        ptv = pt_ps[0:64, 0:256].rearrange("p (h j) -> p h j", h=4)
                        fw.op("dve", lambda e: e.tensor_tensor(out=ptf[:], in0=ptv, in1=wk4.to_broadcast([64, 4, 64]), op=ALU.mult),
                              reads=[b_ptps, b_tokT], writes=[b_ptf])
                        fw.op("dve", lambda e: e.tensor_tensor(out=ptm[:], in0=ptf[:], in1=cm[:].unsqueeze(1).to_broadcast([64, 4, 64]), op=ALU.mult),
                              reads=[b_ptf, b_cm], writes=[b_ptm])
                        hch, b_hch = hchs[si % 2]
                        if d == 1:
                            hfch, b_hfch = hfchs[si % 2]
                            fw.dma("sp", lambda e: e.dma_start(out=hfch[:], in_=hf_d[c * 64:(c + 1) * 64, :].rearrange("p (h d) -> p h d", h=4)),
                                   reads=[b_hf], writes=[b_hfch])
                        for h in range(4):
                            r = d * 4 + h
                            up_, b_up = u_ps[cnt["u"] % 2]
                            cnt["u"] += 1
                            C_, b_C = Cst[h]
                            fw.op("pe", lambda e: e.matmul(up_[:, 0:257], lhsT=kw[:, h, :], rhs=vch[:, h, :], start=True, stop=True),
                                  reads=[b_kw, b_vch], writes=[b_up])
                            fw.op("dve", lambda e: e.scalar_tensor_tensor(out=C_[:], in0=C_[:], scalar=WIbc[:, r, c:c + 1], in1=up_[:, 0:257],
                                                                          op0=ALU.mult, op1=ALU.add), reads=[b_C, b_WIbc, b_up], writes=[b_C])
                        for h in range(4):
                            r = d * 4 + h
                            op_, b_op = o_ps[cnt["o"] % 2]
                            cnt["o"] += 1
                            D_, b_D = Dst[h]
                            C_, b_C = Cst[h]
                            dd, b_dd = dds[h]
                            fw.op("pe", lambda e: e.matmul(op_[0:64, 0:257], lhsT=ptm[:, h, :], rhs=vch[:, h, :], start=True, stop=False),
                                  reads=[b_ptm, b_vch], writes=[b_op])
                            fw.op("pe", lambda e: e.matmul(op_[0:64, 0:257], lhsT=qkc[:, h, :], rhs=D_[:], start=False, stop=True),
                                  reads=[b_qkc, b_D], writes=[b_op])
                            if si + 1 < NCH:
                                cn = ordr[si + 1]
                                fw.op("act", lambda e: e.activation(out=D_[:], in_=C_[:], func=AF.Copy, scale=WIbc[:, r, cn:cn + 1]),
                                      reads=[b_C, b_WIbc], writes=[b_D])
                            fw.op("dve", lambda e: e.tensor_tensor(out=dd[:, 0:1], in0=op_[0:64, 256:257], in1=tokT[:, c, 8 + r:9 + r], op=ALU.max),
                                  reads=[b_op, b_tokT], writes=[b_dd])
                            fw.op("dve", lambda e: e.scalar_tensor_tensor(out=dd[:, 0:1], in0=op_[0:64, 256:257], scalar=-1.0, in1=dd[:, 0:1],
                                                                          op0=ALU.mult, op1=ALU.max), reads=[b_op, b_dd], writes=[b_dd])
                            fw.op("dve", lambda e: e.reciprocal(out=dd[:, 1:2], in_=dd[:, 0:1]), reads=[b_dd], writes=[b_dd])
                            if d == 0:
                                fw.op("act", lambda e: e.activation(out=hch[:, h, :], in_=op_[0:64, 0:256], func=AF.Copy, scale=dd[:, 1:2]),
                                      reads=[b_op, b_dd], writes=[b_hch])
                            else:
                                fw.op("dve", lambda e: e.scalar_tensor_tensor(out=hch[:, h, :], in0=op_[0:64, 0:256], scalar=dd[:, 1:2], in1=hfch[:, h, :],
                                                                              op0=ALU.mult, op1=ALU.add), reads=[b_op, b_dd, b_hfch], writes=[b_hch])
                        if d == 0:
                            fw.dma("sp", lambda e: e.dma_start(out=hf_d[c * 64:(c + 1) * 64, :].rearrange("p (h d) -> p h d", h=4), in_=hch[:]),
                                   reads=[b_hch], writes=[b_hf], waw=False)
                            continue
                        aw, b_aw = aws[si % 2]
                        rs, b_rs = rss[si % 2]
                        fw.op("pool", lambda e: e.tensor_tensor(out=aw[:], in0=hch[:].rearrange("p h d -> p (h d)"), in1=WN[0:64, :], op=ALU.mult),
                              reads=[b_hch, b_WN], writes=[b_aw])
                        for h in range(4):
                            fw.op("act", lambda e: e.activation(out=sqt[:, h, :], in_=hch[:, h, :], func=AF.Square, accum_out=rs[:, h:h + 1]),
                                  reads=[b_hch], writes=[b_sqt, b_rs])
                        fw.op("act", lambda e: e.activation(out=rs[:, 4:8], in_=rs[:, 0:4], func=AF.Ln, scale=1.0 / 256, bias=EPS),
                              reads=[b_rs], writes=[b_rs])
                        fw.op("act", lambda e: e.activation(out=rs[:, 8:12], in_=rs[:, 4:8], func=AF.Exp, scale=-0.5),
                              reads=[b_rs], writes=[b_rs])
                        if pending_ro[0] is not None:
                            pending_ro[0]()

                        def stage2(c=c, si=si, aw=aw, b_aw=b_aw, rs=rs, b_rs=b_rs):
                            t = c // 2
                            off = (c % 2) * 64
                            a16, b_a16 = a16s[si % 2]
                            fw.op("dve", lambda e: e.tensor_tensor(out=a16[:].rearrange("p (h d) -> p h d", h=4),
                                                                   in0=aw[:].rearrange("p (h d) -> p h d", h=4),
                                                                   in1=rs[:, 8:12].unsqueeze(2).to_broadcast([64, 4, 256]), op=ALU.mult),
                                  reads=[b_aw, b_rs], writes=[b_a16])
                            for kk in range(8):
                                fw.op("pe", lambda e: e.transpose(aT_ps[:, kk, off:off + 64], a16[:, kk * 128:(kk + 1) * 128], self.ident_bf[0:64, 0:64]),
                                      reads=[b_a16, self.b_ident], writes=[b_aTps])
                            if c % 2 == 1:
                                return
                            aT, b_aT = aTs[cnt["t"] % 2]
                            oTt, b_oTt = oTts[cnt["t"] % 2]
                            xt, b_xt = xts[cnt["t"] % 2]
                            tmp, b_tmp = tmps[cnt["t"] % 2]
                            cnt["t"] += 1
                            fw.dma("sp", lambda e: e.dma_start(out=oTt[:], in_=oT_d[t]), reads=[b_oT], writes=[b_oTt])
                            rap, b_rap = self.res_tile(bi, t)
                            fw.dma("sp", lambda e: e.dma_start(out=xt[:], in_=rap), reads=[b_rap], writes=[b_xt])
                            fw.op("dve", lambda e: e.tensor_tensor(out=aT[:], in0=aT_ps[:], in1=oTt[:], op=ALU.mult), reads=[b_aTps, b_oTt], writes=[b_aT])
                            grow, b_grow = g1[2 if t < 2 else bi]
                            for half in range(2):
                                for kk in range(8):
                                    fw.op("pe", lambda e: e.matmul(y_ps[:], lhsT=aT[:, kk, :], rhs=wout[:, kk, half * 512:(half + 1) * 512],
                                                                   start=(kk == 0), stop=(kk == 7)), reads=[b_aT, b_wout], writes=[b_yps])
                                hs = slice(half * 512, (half + 1) * 512)
                                fw.op("dve", lambda e: e.tensor_tensor(out=tmp[:, hs], in0=y_ps[:], in1=grow[:, hs], op=ALU.mult),
                                      reads=[b_yps, b_grow], writes=[b_tmp])
                                fw.op("pool", lambda e: e.tensor_tensor(out=tmp[:, hs], in0=tmp[:, hs], in1=xt[:, hs], op=ALU.add),
                                      reads=[b_tmp, b_xt], writes=[b_tmp])
                            fw.dma("sp", lambda e: e.dma_start(out=rap, in_=tmp[:]), reads=[b_tmp], writes=[b_rap], waw=False)
                        pending_ro[0] = stage2
                    if pending_ro[0] is not None:
                        pending_ro[0]()
                        pending_ro[0] = None

    def phase_final(self):
        fw = self.fw
        with fw.scope() as sc:
            W = self.norm_work(sc)
            nf, b_nf = self.load_row(sc, "nf", self.norm_final[0:1, :])
            zr, b_zr = sc.sb("zrow", [128, D], F32)
            fw.op("pool", lambda e: e.memset(zr[:], 0.0), writes=[b_zr])
            outs = [sc.sb("fo", [128, D], F32) for _ in range(2)]
            tiles = [(bi, t) for bi in range(self.nb) for t in range(NL // 128)]
            nxt_x = self.norm_load(W, self.outs[tiles[0][0]][0:128, :], self.b_outs[tiles[0][0]])
            for i, (bi, t) in enumerate(tiles):
                cur_x = nxt_x
                if i + 1 < len(tiles):
                    b2, t2 = tiles[i + 1]
                    nxt_x = self.norm_load(W, self.outs[b2][t2 * 128:(t2 + 1) * 128, :], self.b_outs[b2])
                o, b_o = outs[i % 2]
                self.norm_tile(W, cur_x, nf, b_nf, zr, b_zr, o[:], b_o)
                fw.dma("sp", lambda e: e.dma_start(out=self.outs[bi][t * 128:(t + 1) * 128, :], in_=o[:]),
                       reads=[b_o], writes=[self.b_outs[bi]], waw=False)


def rope_tables():
    grid_w = 64
    pairs = 16
    t = np.arange(NL)
    row = (t // grid_w).astype(np.float32)
    col = (t % grid_w).astype(np.float32)
    inv = (np.float32(10000.0) ** (-np.arange(pairs, dtype=np.float32) / np.float32(pairs))).astype(np.float32)
    ang = np.concatenate([row[:, None] * inv, col[:, None] * inv], axis=-1).astype(np.float32)
    cos = np.cos(ang).astype(np.float32).T
    sin = np.sin(ang).astype(np.float32).T
    cos64 = np.concatenate([cos, cos], axis=0)
    sin64 = np.concatenate([sin, sin], axis=0)
    return (np.ascontiguousarray(np.concatenate([cos64, cos64], axis=0)),
            np.ascontiguousarray(np.concatenate([sin64, sin64], axis=0)))


FULL_PLAN = [("mod",)] + [s for l in range(DEPTH) for s in (("mix", l), ("moe", l))] + [("final",)]


def make_in_maps(inputs, n_cores, nb):
    cos, sin = rope_tables()
    shared = {k: np.ascontiguousarray(v) for k, v in inputs.items() if k not in ("x", "c", "ctx", "c_ctx", "norm_final")}
    shared["c_ctx"] = np.ascontiguousarray(inputs["c_ctx"]).reshape(1, D)
    shared["norm_final"] = np.ascontiguousarray(inputs["norm_final"]).reshape(1, D)
    shared["rope_cos"] = cos
    shared["rope_sin"] = sin
    maps = []
    for c in range(n_cores):
        m = dict(shared)
        m["x"] = np.ascontiguousarray(inputs["x"][c * nb:(c + 1) * nb])
        m["c"] = np.ascontiguousarray(inputs["c"][c * nb:(c + 1) * nb])
        m["ctx"] = np.ascontiguousarray(inputs["ctx"][c * nb:(c + 1) * nb])
        maps.append(m)
    return maps


def kernel(**inputs):
    n_cores = 8
    nb = 2
    prog = Prog(nb, FULL_PLAN)
    maps = make_in_maps(inputs, n_cores, nb)
    res = run_bass_kernel_spmd(prog.nc, maps, core_ids=list(range(n_cores)))
    return np.stack([r[f"out{b}"] for r in res.results for b in range(nb)], axis=0).astype(np.float32)
```
